# Optimizing a Trainium2 kernel written in Bass

```python
import jax, jax.numpy as jnp
from jax import lax
import numpy as np

D_MODEL = 1024
BATCH = 2
SEQ = 16384
DEPTH = 2

CHUNK = 64
EPS = 1e-6
ROPE_BASE = 10000.0
RET_HEADS = 4
RET_DK = 128
RET_DV = 128
RET_WIDTH = RET_HEADS * RET_DV
CONV_CH = 512
CONV_GROUPS = 8
CONV_K = 31
SG_CH = 512
SG_GROUPS = 4
SG_BLOCK = 128
SC_CH = 512
SC_GROUPS = 8
SC_K = 3
D_FF = 2816
N_EXPERTS = 8
TOP_K = 2
D_EXPERT = 3584
MOE_BLOCK = 256
EVEN_IN = 2 * RET_HEADS * RET_DK + 2 * RET_WIDTH + 2 * CONV_CH
ODD_IN = 2 * SG_CH + 3 * SC_CH
N_EVEN = (DEPTH + 1) // 2
N_ODD = DEPTH // 2

kernel_name = 'hybrid_retention_conformer_gmlp_shortconv_moe'


def rmsnorm(x, g):
    x32 = x.astype(jnp.float32)
    y = x32 * lax.rsqrt(jnp.mean(x32 * x32, axis=-1, keepdims=True) + EPS)
    return y.astype(x.dtype) * g


def layernorm(x, g, b):
    x32 = x.astype(jnp.float32)
    mu = jnp.mean(x32, axis=-1, keepdims=True)
    var = jnp.mean(jnp.square(x32 - mu), axis=-1, keepdims=True)
    y = (x32 - mu) * lax.rsqrt(var + EPS)
    return y.astype(x.dtype) * g + b


def rotary(x, pos):
    half = x.shape[-1] // 2
    freqs = ROPE_BASE ** (-jnp.arange(half, dtype=jnp.float32) / half)
    ang = pos[:, None] * freqs[None, :]
    cos = jnp.cos(ang)[None, :, None, :]
    sin = jnp.sin(ang)[None, :, None, :]
    x32 = x.astype(jnp.float32)
    x1, x2 = x32[..., :half], x32[..., half:]
    return jnp.concatenate([x1 * cos - x2 * sin, x2 * cos + x1 * sin], axis=-1)


def causal_depthwise_conv(x, w):
    k = w.shape[0]
    return lax.conv_general_dilated(
        x, w[:, None, :].astype(x.dtype), window_strides=(1,), padding=[(k - 1, 0)],
        dimension_numbers=('NWC', 'WIO', 'NWC'), feature_group_count=x.shape[-1])


def chunk_retention(q, k, v):
    bn, s, h, dk = q.shape
    dv = v.shape[-1]
    n_chunks = s // CHUNK

    def to_chunks(t):
        return t.reshape(bn, n_chunks, CHUNK, h, t.shape[-1]).transpose(1, 0, 3, 2, 4)

    log_g = jnp.log1p(-jnp.power(2.0, -5.0 - jnp.arange(h, dtype=jnp.float32)))
    idx = jnp.arange(CHUNK, dtype=jnp.float32)
    intra = jnp.exp(log_g[:, None, None] * jnp.abs(idx[:, None] - idx[None, :]))
    q_dec = jnp.exp(log_g[:, None] * (idx + 1.0))[:, :, None]
    k_dec = jnp.exp(log_g[:, None] * (CHUNK - 1.0 - idx))[:, :, None]
    c_dec = jnp.exp(log_g * CHUNK)[:, None, None]

    def step(state, inp):
        qc, kc, vc = inp
        scores = jnp.einsum('bhcd,bhld->bhcl', qc, kc) * intra
        out = (jnp.einsum('bhcl,bhle->bhce', scores, vc)
               + jnp.einsum('bhcd,bhde->bhce', qc * q_dec, state))
        state = c_dec * state + jnp.einsum('bhld,bhle->bhde', kc * k_dec, vc)
        return state, out

    init = jnp.zeros((bn, h, dk, dv), jnp.float32)
    _, o = lax.scan(step, init, (to_chunks(q), to_chunks(k), to_chunks(v)))
    return o.transpose(1, 0, 3, 2, 4).reshape(bn, s, h, dv)


def even_mixer(hn, w_in, ret_gn, dw_w, dw_b, cv_ln_g, cv_ln_b, w_out, pos):
    bn, s, _ = hn.shape
    nq = RET_HEADS * RET_DK
    z = hn @ w_in
    q, k, v, g, a, b = jnp.split(
        z, [nq, 2 * nq, 2 * nq + RET_WIDTH, 2 * nq + 2 * RET_WIDTH,
            2 * nq + 2 * RET_WIDTH + CONV_CH], axis=-1)
    q = rotary(q.reshape(bn, s, RET_HEADS, RET_DK), pos) * (RET_DK ** -0.5)
    k = rotary(k.reshape(bn, s, RET_HEADS, RET_DK), pos)
    v = v.reshape(bn, s, RET_HEADS, RET_DV).astype(jnp.float32)
    r = chunk_retention(q, k, v)
    mu = jnp.mean(r, axis=-1, keepdims=True)
    var = jnp.mean(jnp.square(r - mu), axis=-1, keepdims=True)
    r = ((r - mu) * lax.rsqrt(var + EPS)).reshape(bn, s, RET_WIDTH).astype(hn.dtype) * ret_gn
    y_ret = jax.nn.silu(g) * r
    c = a * jax.nn.sigmoid(b)
    c = causal_depthwise_conv(c, dw_w) + dw_b
    y_cv = jax.nn.silu(layernorm(c, cv_ln_g, cv_ln_b))
    return jnp.concatenate([y_ret, y_cv], axis=-1) @ w_out


def odd_mixer(hn, w_in, sg_ln_g, sg_ln_b, sg_w, sg_b, sc_w, w_out):
    bn, s, _ = hn.shape
    z = hn @ w_in
    zs, bg, cg, hv = jnp.split(z, [2 * SG_CH, 2 * SG_CH + SC_CH, 2 * SG_CH + 2 * SC_CH], axis=-1)
    zs = jax.nn.gelu(zs)
    u, v = jnp.split(zs, 2, axis=-1)
    v = layernorm(v, sg_ln_g, sg_ln_b)
    n_blk = s // SG_BLOCK
    vb = v.reshape(bn, n_blk, SG_BLOCK, SG_GROUPS, SG_CH // SG_GROUPS)
    pchunk = jnp.arange(SG_BLOCK) // CHUNK
    mask = (pchunk[:, None] >= pchunk[None, :]).astype(sg_w.dtype)
    w_s = sg_w * mask[None]
    sp = jnp.einsum('gpq,bmqgc->bmpgc', w_s, vb) + sg_b.T[None, None, :, :, None]
    y_sg = u * sp.reshape(bn, s, SG_CH)
    y_sc = bg * causal_depthwise_conv(cg * hv, sc_w)
    return jnp.concatenate([y_sg, y_sc], axis=-1) @ w_out


def swiglu(hn, w_gate, w_up, w_down):
    return (jax.nn.silu(hn @ w_gate) * (hn @ w_up)) @ w_down


def moe_swiglu(hn, router, e_gate, e_up, e_down):
    bn, s, d = hn.shape
    t = bn * s
    a = t * TOP_K
    xt = hn.reshape(t, d)
    logits = (xt @ router).astype(jnp.float32)
    top_v, top_i = lax.top_k(logits, TOP_K)
    gates = jax.nn.softmax(top_v, axis=-1)
    e_flat = top_i.reshape(-1)
    tok_flat = jnp.repeat(jnp.arange(t, dtype=jnp.int32), TOP_K)
    w_flat = gates.reshape(-1)
    order = jnp.argsort(e_flat)
    se, stok, sw = e_flat[order], tok_flat[order], w_flat[order]
    counts = jnp.bincount(e_flat, length=N_EXPERTS)
    start = jnp.cumsum(counts) - counts
    pcounts = (counts + MOE_BLOCK - 1) // MOE_BLOCK * MOE_BLOCK
    pend = jnp.cumsum(pcounts)
    pstart = pend - pcounts
    dest = pstart[se] + jnp.arange(a) - start[se]
    rows = ((a + MOE_BLOCK - 1) // MOE_BLOCK) * MOE_BLOCK + N_EXPERTS * MOE_BLOCK
    n_blocks = rows // MOE_BLOCK
    row_tok = jnp.zeros((rows,), jnp.int32).at[dest].set(stok)
    row_w = jnp.zeros((rows,), jnp.float32).at[dest].set(sw)
    blk_e = jnp.minimum(jnp.searchsorted(pend, jnp.arange(n_blocks) * MOE_BLOCK, side='right'),
                        N_EXPERTS - 1)
    xg = xt[row_tok].reshape(n_blocks, MOE_BLOCK, d)

    def expert_block(args):
        xb, e = args
        hb = jax.nn.silu(xb @ e_gate[e]) * (xb @ e_up[e])
        return hb @ e_down[e]

    yb = lax.map(expert_block, (xg, blk_e)).reshape(rows, d)
    yb = yb * row_w[:, None].astype(hn.dtype)
    out = jnp.zeros((t, d), hn.dtype).at[row_tok].add(yb)
    return out.reshape(bn, s, d)


def setup_inputs(seed: int = 0) -> dict:
    key = jax.random.key(seed)
    ks = iter(jax.random.split(key, 40))

    def dense(shape, fan_in, scale=1.0):
        return jax.random.normal(next(ks), shape, jnp.float32) * (scale * fan_in ** -0.5)

    def gain(shape):
        return 1.0 + 0.02 * jax.random.normal(next(ks), shape, jnp.float32)

    def bias(shape, scale=0.02):
        return scale * jax.random.normal(next(ks), shape, jnp.float32)

    ne, no = N_EVEN, N_ODD
    return {
        'x': jax.random.normal(next(ks), (BATCH, SEQ, D_MODEL), jnp.float32),
        'e_norm_mix': gain((ne, D_MODEL)),
        'e_w_in': dense((ne, D_MODEL, EVEN_IN), D_MODEL),
        'e_ret_gn': gain((ne, RET_WIDTH)),
        'e_dw_w': dense((ne, CONV_K, CONV_CH), CONV_K),
        'e_dw_b': bias((ne, CONV_CH)),
        'e_cv_ln_g': gain((ne, CONV_CH)),
        'e_cv_ln_b': bias((ne, CONV_CH)),
        'e_w_out': dense((ne, RET_WIDTH + CONV_CH, D_MODEL), RET_WIDTH + CONV_CH),
        'e_norm_ffn': gain((ne, D_MODEL)),
        'e_ffn_gate': dense((ne, D_MODEL, D_FF), D_MODEL),
        'e_ffn_up': dense((ne, D_MODEL, D_FF), D_MODEL),
        'e_ffn_down': dense((ne, D_FF, D_MODEL), D_FF),
        'o_norm_mix': gain((no, D_MODEL)),
        'o_w_in': dense((no, D_MODEL, ODD_IN), D_MODEL),
        'o_sg_ln_g': gain((no, SG_CH)),
        'o_sg_ln_b': bias((no, SG_CH)),
        'o_sg_w': dense((no, SG_GROUPS, SG_BLOCK, SG_BLOCK), SG_BLOCK, 0.5),
        'o_sg_b': 1.0 + bias((no, SG_GROUPS, SG_BLOCK), 0.1),
        'o_sc_w': dense((no, SC_K, SC_CH), SC_K),
        'o_w_out': dense((no, SG_CH + SC_CH, D_MODEL), SG_CH + SC_CH),
        'o_norm_ffn': gain((no, D_MODEL)),
        'o_router': dense((no, D_MODEL, N_EXPERTS), D_MODEL),
        'o_exp_gate': dense((no, N_EXPERTS, D_MODEL, D_EXPERT), D_MODEL),
        'o_exp_up': dense((no, N_EXPERTS, D_MODEL, D_EXPERT), D_MODEL),
        'o_exp_down': dense((no, N_EXPERTS, D_EXPERT, D_MODEL), D_EXPERT),
        'final_norm': gain((D_MODEL,)),
    }


def reference(x, e_norm_mix, e_w_in, e_ret_gn, e_dw_w, e_dw_b, e_cv_ln_g, e_cv_ln_b, e_w_out,
              e_norm_ffn, e_ffn_gate, e_ffn_up, e_ffn_down,
              o_norm_mix, o_w_in, o_sg_ln_g, o_sg_ln_b, o_sg_w, o_sg_b, o_sc_w, o_w_out,
              o_norm_ffn, o_router, o_exp_gate, o_exp_up, o_exp_down, final_norm):
    pos = jnp.arange(x.shape[1], dtype=jnp.float32)
    h = x
    for i in range(DEPTH):
        l = i // 2
        if i % 2 == 0:
            h = h + even_mixer(rmsnorm(h, e_norm_mix[l]), e_w_in[l], e_ret_gn[l], e_dw_w[l],
                               e_dw_b[l], e_cv_ln_g[l], e_cv_ln_b[l], e_w_out[l], pos)
            h = h + swiglu(rmsnorm(h, e_norm_ffn[l]), e_ffn_gate[l], e_ffn_up[l], e_ffn_down[l])
        else:
            h = h + odd_mixer(rmsnorm(h, o_norm_mix[l]), o_w_in[l], o_sg_ln_g[l], o_sg_ln_b[l],
                              o_sg_w[l], o_sg_b[l], o_sc_w[l], o_w_out[l])
            h = h + moe_swiglu(rmsnorm(h, o_norm_ffn[l]), o_router[l], o_exp_gate[l],
                               o_exp_up[l], o_exp_down[l])
    return rmsnorm(h, final_norm)
```

```python
import numpy as np
import concourse.bass as bass
import concourse.mybir as mybir
from concourse.bass_utils import run_bass_kernel_spmd
from contextlib import ExitStack

F32 = mybir.dt.float32
BF16 = mybir.dt.bfloat16
AF = mybir.ActivationFunctionType
ALU = mybir.AluOpType

P = 128
KC = 8
D = 1024
TW = 512
HALO = 128
ST_TILES = 2
D_FF = 2816
D_EXP = 3584
NEXP = 8
EPS = 1e-6
CONV_K = 31
GELU_C = 1.5957691216057308

VEC = {}
_o = 0
for _n, _w in (("e_norm_mix", 8), ("e_ret_gn", 4), ("e_dw_b", 4), ("e_cv_ln_g", 4), ("e_cv_ln_b", 4),
               ("e_norm_ffn", 8), ("o_norm_mix", 8), ("o_sg_ln_g", 4), ("o_sg_ln_b", 4),
               ("o_norm_ffn", 8), ("final_norm", 8), ("e_dw_w", 124), ("o_sc_w", 12)):
    VEC[_n] = _o
    _o += _w
NVEC = _o


class Buf:
    def __init__(self, name):
        self.name = name
        self.writers = {}
        self.dma_writers = []
        self.readers = {}
        self.dma_readers = []


class SemCounter:
    def __init__(self, sem):
        self.sem = sem
        self.count = 0


class Op:
    __slots__ = ("eng", "fn", "deps", "is_dma", "semc", "semval", "milestone", "idx")

    def __init__(self, eng, fn, is_dma=False, semc=None):
        self.eng = eng
        self.fn = fn
        self.deps = []
        self.is_dma = is_dma
        self.semc = semc
        self.semval = None
        self.milestone = False
        self.idx = None


COMPUTE = ("pe", "act", "dve")
ENGS = ("pe", "act", "dve", "pool", "sp")


class Prog:
    def __init__(self, nc, es):
        self.nc = nc
        self.es = es
        self.ops = {e: [] for e in ENGS}
        self.nsem = 0

    def new_semc(self):
        self.nsem += 1
        return SemCounter(self.es.enter_context(self.nc.semaphore("s%d" % self.nsem)))

    def op(self, eng, fn, reads=(), writes=(), dma_sem=None):
        is_dma = dma_sem is not None
        o = Op(eng, fn, is_dma, dma_sem)
        deps = []
        for b in reads:
            for e, w in b.writers.items():
                deps.append((w, "raw"))
            for w in b.dma_writers:
                deps.append((w, "raw"))
        for b in writes:
            for e, w in b.writers.items():
                deps.append((w, "waw"))
            for w in b.dma_writers:
                deps.append((w, "waw"))
            for e, r in b.readers.items():
                deps.append((r, "war"))
            for r in b.dma_readers:
                deps.append((r, "war"))
        seen = set()
        for d, kind in deps:
            if d is o or id(d) in seen:
                continue
            if (not d.is_dma) and (not is_dma) and d.eng == eng:
                if kind != "raw" or eng == "pe":
                    continue
            if d.is_dma and is_dma and d.semc is dma_sem and kind == "waw":
                continue
            seen.add(id(d))
            o.deps.append(d)
            d.milestone = True
        for b in writes:
            b.writers = {}
            b.dma_writers = []
            b.readers = {}
            b.dma_readers = []
            if is_dma:
                b.dma_writers.append(o)
            else:
                b.writers[eng] = o
        for b in reads:
            if b in writes:
                continue
            if is_dma:
                b.dma_readers.append(o)
            else:
                b.readers[eng] = o
        if is_dma:
            dma_sem.count += 16
            o.semval = dma_sem.count
        self.ops[eng].append(o)
        return o

    def emit(self):
        nc = self.nc
        EPOCH = 16000
        esems = {}
        for e in COMPUTE:
            n = 0
            for o in self.ops[e]:
                if o.milestone:
                    ep = n // EPOCH
                    if (e, ep) not in esems:
                        esems[(e, ep)] = self.es.enter_context(nc.semaphore("eng_%s_%d" % (e, ep)))
                    o.semc = esems[(e, ep)]
                    n += 1
                    o.semval = n - ep * EPOCH
            print("engine", e, "ops", len(self.ops[e]), "milestones", n)
        print("pool ops", len(self.ops["pool"]), "sp ops", len(self.ops["sp"]), "sems", self.nsem + len(esems))
        block = self.es.enter_context(nc.Block())

        def run(engname, eobj):
            waited = {}
            for o in self.ops[engname]:
                need = {}
                for d in o.deps:
                    sem = d.semc.sem if d.is_dma else d.semc
                    k = id(sem)
                    if k not in need or need[k][1] < d.semval:
                        need[k] = (sem, d.semval)
                for k, (sem, val) in need.items():
                    if waited.get(k, 0) < val:
                        eobj.wait_ge(sem, val)
                        waited[k] = val
                ins = o.fn(eobj)
                if o.is_dma:
                    ins.then_inc(o.semc.sem, 16)
                elif o.milestone:
                    ins.then_inc(o.semc, 1)

        @block.tensor
        def _(e):
            run("pe", e)

        @block.scalar
        def _(e):
            run("act", e)

        @block.vector
        def _(e):
            run("dve", e)

        @block.gpsimd
        def _(e):
            run("pool", e)

        @block.sync
        def _(e):
            run("sp", e)


class Rot:
    def __init__(self, items):
        self.items = items
        self.i = 0

    def next(self):
        it = self.items[self.i % len(self.items)]
        self.i += 1
        return it


def build_program(PRE, MAIN, CAP, dbg=False):
    NTOK = PRE + HALO + MAIN
    NSLOT = NEXP * CAP
    ZROW = NSLOT
    NBLKC = MAIN // P
    CB = CAP // P
    NST = MAIN // (ST_TILES * TW)
    HW = HALO + ST_TILES * TW
    nc = bass.Bass("TRN2", target_bir_lowering=False)

    def din(name, shape, dt=F32):
        return nc.dram_tensor(name, list(shape), dt, kind="ExternalInput").ap()

    xT = din("xT", [P, KC, NTOK])
    cosT = din("cosT", [P, NTOK])
    sinT = din("sinT", [P, NTOK])
    vecs_d = din("vecs", [P, NVEC])
    maskT_d = din("maskT", [P, 4, P])
    qdec_d = din("qdec", [P, 4, P])
    kdec_d = din("kdec", [P, 4])
    ident_d = din("ident", [P, P])
    sgmask_d = din("sgmask", [P, P])
    sgwT_d = din("sgwT", [P, 4, P])
    sgb_d = din("sgb", [P, 4, P])
    halom_d = din("halom", [P, 1])
    e_w_in = din("e_w_in", [D, 3072])
    e_w_out = din("e_w_out", [D, D])
    e_ffn_gate = din("e_ffn_gate", [D, D_FF])
    e_ffn_up = din("e_ffn_up", [D, D_FF])
    e_ffn_down = din("e_ffn_down", [D_FF, D])
    o_w_in = din("o_w_in", [D, 2560])
    o_w_out = din("o_w_out", [D, D])
    o_router = din("o_router", [D, NEXP])
    o_exp_gate = din("o_exp_gate", [NEXP, D, D_EXP])
    o_exp_up = din("o_exp_up", [NEXP, D, D_EXP])
    o_exp_down = din("o_exp_down", [NEXP, D_EXP, D])
    fng_d = din("fng", [P, D])
    ut_d = din("ut", [P, P])
    eoff_d = din("eoff", [P, NEXP])
    out_d = nc.dram_tensor("out", [MAIN, D], F32, kind="ExternalOutput").ap()
    dk = dict(kind="ExternalOutput") if dbg else {}
    XG = nc.dram_tensor("xg_scratch", [NSLOT + 72, D], BF16, **dk).ap()
    YS = nc.dram_tensor("ys_scratch", [NSLOT + 40, D], F32, **dk).ap()
    H3D = nc.dram_tensor("h3_scratch", [MAIN, D], F32, **dk).ap()
    if dbg:
        dbg_idx = nc.dram_tensor("dbg_idx", [P, 2, NBLKC], mybir.dt.uint32, kind="ExternalOutput").ap()
        dbg_w = nc.dram_tensor("dbg_w", [P, 2, NBLKC], F32, kind="ExternalOutput").ap()

    cdec = [float((1.0 - 2.0 ** (-5 - h)) ** 128) for h in range(4)]

    with ExitStack() as es:
        pg = Prog(nc, es)

        def sb(name, shape, dt):
            return es.enter_context(nc.sbuf_tensor(name, list(shape), dt))

        ARENA_N = 61696
        ARENA = sb("ARENA", [P, ARENA_N], BF16)
        _ao = [0]
        P1_BUFS = []
        P2_BUFS = []

        def ar(shape, dt):
            n = int(np.prod(shape[1:])) * (2 if dt == F32 else 1)
            assert _ao[0] + n <= ARENA_N, (_ao[0], n)
            a = ARENA[:, _ao[0]:_ao[0] + n]
            _ao[0] += n
            if dt == F32:
                a = a.bitcast(F32)
            if len(shape) == 3:
                a = a.rearrange("p (a b) -> p a b", a=shape[1])
            return a

        def atmp(name, shape, dt, lst=None):
            b = Buf(name)
            (P1_BUFS if lst is None else lst).append(b)
            return ar(shape, dt), b

        HB = ar([P, KC, HW], F32)
        HNB = ar([P, KC, HW], BF16)
        slot_off = [0] + [HALO + i * TW for i in range(ST_TILES)]
        slot_w = [HALO] + [TW] * ST_TILES
        Hbuf = [Buf("H%d" % i) for i in range(1 + ST_TILES)]
        HNbuf = [Buf("HN%d" % i) for i in range(1 + ST_TILES)]
        P1_BUFS.extend(Hbuf + HNbuf)
        Hsem = [pg.new_semc() for _ in range(1 + ST_TILES)]

        def Hs(s, W=None):
            W = slot_w[s] if W is None else W
            return HB[:, :, slot_off[s]:slot_off[s] + W]

        def HNs(s, W=None):
            W = slot_w[s] if W is None else W
            return HNB[:, :, slot_off[s]:slot_off[s] + W]

        WBUF = sb("WBUF", [P, KC * 3072], BF16)
        WIN0 = WBUF[:, :].rearrange("p (c n) -> p c n", c=KC)
        WIN1 = WBUF[:, 0:KC * 2560].rearrange("p (c n) -> p c n", c=KC)
        WOUT = ar([P, KC, D], BF16)
        WINb, WOUTb = Buf("WIN"), Buf("WOUT")
        P1_BUFS.append(WOUTb)
        WINsem, WOUTsem = pg.new_semc(), pg.new_semc()
        G = 2
        FSZ = 3 * KC * G * P
        FS = []
        for i in range(2):
            o0 = i * FSZ
            FS.append(dict(g=WBUF[:, o0:o0 + 2048].rearrange("p (c n) -> p c n", c=KC),
                           u=WBUF[:, o0 + 2048:o0 + 4096].rearrange("p (c n) -> p c n", c=KC),
                           d=WBUF[:, o0 + 4096:o0 + 6144].rearrange("p (j n) -> p j n", j=G),
                           buf=Buf("FS%d" % i), sem=pg.new_semc()))
        fs_rot = Rot(FS)
        _wo = [2 * FSZ]

        def carve(n_bf16, dt=BF16):
            a = WBUF[:, _wo[0]:_wo[0] + n_bf16]
            _wo[0] += n_bf16
            return a if dt == BF16 else a.bitcast(F32)

        VECS = sb("VECS", [P, NVEC], F32)
        MASKT = sb("MASKT", [P, 4, P], F32)
        QDEC = sb("QDEC", [P, 4, P], F32)
        KDEC = sb("KDEC", [P, 4], F32)
        IDB = sb("IDB", [P, P], BF16)
        IDF = sb("IDF", [P, P], F32)
        ONES = sb("ONES", [P, P], F32)
        EPSC = sb("EPSC", [P, 1], F32)
        SGW = sb("SGW", [P, 4, P], BF16)
        SGMASK = sb("SGMASK", [P, P], F32)
        SGB = sb("SGB", [P, 4, P], F32)
        HALOM = sb("HALOM", [P, 1], F32)
        RG = sb("RG", [P, KC, NEXP], F32)
        CONST = Buf("CONST")
        constsem = pg.new_semc()

        def tmp(name, shape, dt):
            return sb(name, shape, dt), Buf(name)

        SQ = [tmp("SQ%d" % i, [P, TW], F32) for i in range(2)]
        sq_rot = Rot(SQ)
        RSTD, RSTDb = tmp("RSTD", [P, TW], F32)
        TA = [tmp("TA%d" % i, [P, TW], F32) for i in range(2)]
        ta_rot = Rot(TA)
        TBt = [tmp("TB%d" % i, [P, TW], F32) for i in range(2)]
        tb_rot = Rot(TBt)
        MEAN, MEANb = tmp("MEAN", [P, TW], F32)
        VAR, VARb = tmp("VAR", [P, TW], F32)
        ROTQ, ROTQb = atmp("ROTQ", [P, 4, TW], BF16)
        _off_rotk = _ao[0]
        ROTK, ROTKb = atmp("ROTK", [P, 4, TW], BF16)
        QD, QDb = atmp("QD", [P, 4, TW], BF16)
        KD, KDb = atmp("KD", [P, 4, TW], BF16)
        MBUF = ARENA[:, _off_rotk:_off_rotk + 4 * (2 + TW) * 2].bitcast(F32).rearrange("p (a b) -> p a b", a=4)
        MBb = [ROTKb, QDb, KDb]
        VT, VTb = atmp("VT", [P, 4, TW], BF16)
        PT = [atmp("PT%d" % i, [P, 4, P], BF16) for i in range(2)]
        pt_rot = Rot(PT)
        SILUG, SILUGb = atmp("SILUG", [P, 4, TW], BF16)
        STATE, STATEb = tmp("STATE", [P, 4, P], F32)
        STATEB, STATEBb = tmp("STATEB", [P, 4, P], BF16)
        CBUF, CBUFb = atmp("CBUF", [P, 4, 30 + TW], BF16)
        DG = [atmp("DG%d" % i, [P, P], BF16) for i in range(4)]
        dg_rot = Rot(DG)
        CARRY0, CARRY0b = tmp("CARRY0", [P, 4, 30], BF16)
        CARRY1, CARRY1b = tmp("CARRY1", [P, 4, 2], F32)
        CONVO = ar([P, 4, TW], F32)
        CONVOb = [Buf("CONVO%d" % i) for i in range(4)]
        P1_BUFS.extend(CONVOb)
        COS = [atmp("COS%d" % i, [P, TW], F32) for i in range(2)]
        SIN = [atmp("SIN%d" % i, [P, TW], F32) for i in range(2)]
        tabsem = [(pg.new_semc(), pg.new_semc()) for _ in range(2)]
        AB = [(carve(G * TW).rearrange("p (j n) -> p j n", j=G), Buf("AB%d" % i)) for i in range(2)]
        ab_rot = Rot(AB)
        SGT = [(carve(TW), Buf("SGT%d" % i)) for i in range(2)]
        sgt_rot = Rot(SGT)
        HNT = [(carve(D), Buf("HNT%d" % i)) for i in range(2)]
        hnt_rot = Rot(HNT)
        H3S = [(carve(2 * D, F32), Buf("H3S%d" % i)) for i in range(2)]
        h3s_rot = Rot(H3S)
        hnt_sem = [pg.new_semc() for _ in range(2)]
        IDXA, IDXAb = tmp("IDXA", [P, NBLKC], mybir.dt.uint32)
        IDXB, IDXBb = tmp("IDXB", [P, NBLKC], mybir.dt.uint32)
        WA, WAb = tmp("WA", [P, NBLKC], F32)
        WB, WBb = tmp("WB", [P, NBLKC], F32)
        RUN, RUNb = tmp("RUN", [P, NEXP], F32)
        UT = sb("UT", [P, P], F32)
        EOFF = sb("EOFF", [P, NEXP], F32)
        XGb, YSb, H3Db = Buf("XG"), Buf("YS"), Buf("H3D")
        xg_sem, ys_sem, h3d_sem = pg.new_semc(), pg.new_semc(), pg.new_semc()
        LG, LGb = tmp("LG", [P, 16], F32)
        RT = {n: tmp("RT_" + n, [P, 8], F32) for n in ("L", "MK1", "L2", "MK2", "G", "POS", "OK", "SL", "T8")}
        RS = {n: tmp("RS_" + n, [P, 1], F32) for n in ("M1", "M2", "D", "E", "W1", "W2", "SA", "SB", "SS")}
        FFN_BUFS = [f["buf"] for f in FS] + [b for _, b in AB] + [b for _, b in SGT] + [b for _, b in HNT] + [b for _, b in H3S]
        FENCE = sb("FENCE", [P, 2], F32)

        def fence(reads, writes):
            pg.op("dve", lambda e: e.memset(FENCE[0:1, 0:1], 0.0), reads, writes)
        E0 = sb("E0", [P, 1], F32)

        PS = [es.enter_context(nc.psum_tensor("ps%d" % i, [P, TW], F32)) for i in range(8)]
        PSb = [Buf("ps%d" % i) for i in range(8)]
        mm_rot = Rot([(PS[i], PSb[i]) for i in range(4)])
        aux_rot = Rot([(PS[i], PSb[i]) for i in (4, 5)])
        acc_rot = Rot([(PS[i], PSb[i]) for i in (6, 7)])
        dn_rot = Rot([(PS[i], PSb[i]) for i in (4, 5, 6, 7)])

        def mm(out, lhsT, rhs, start, stop, reads, writes):
            pg.op("pe", lambda e: e.matmul(out, lhsT, rhs, start=start, stop=stop), reads, writes)

        def tr(out, in_, reads, writes):
            pg.op("pe", lambda e: e.transpose(out, in_, IDB[:]), reads + [CONST], writes)

        def act(out, in_, func, reads, writes, bias=None, scale=None):
            kw = {}
            if bias is not None:
                kw["bias"] = bias
            if scale is not None:
                kw["scale"] = scale
            pg.op("act", lambda e: e.activation(out, in_, func, **kw), reads, writes)

        def tt(out, in0, in1, op, reads, writes):
            pg.op("dve", lambda e: e.tensor_tensor(out, in0, in1, op), reads, writes)

        def ts(out, in0, s1, s2, op0, op1, reads, writes):
            if op1 is None:
                pg.op("dve", lambda e: e.tensor_scalar(out, in0, s1, None, op0), reads, writes)
            else:
                pg.op("dve", lambda e: e.tensor_scalar(out, in0, s1, s2, op0, op1), reads, writes)

        def stt(out, in0, scalar, in1, op0, op1, reads, writes):
            pg.op("dve", lambda e: e.scalar_tensor_tensor(out, in0, scalar, in1, op0, op1), reads, writes)

        def vcopy(out, in_, reads, writes):
            pg.op("dve", lambda e: e.tensor_copy(out, in_), reads, writes)

        def recip(out, in_, reads, writes):
            pg.op("dve", lambda e: e.reciprocal(out, in_), reads, writes)

        def memset(out, val, writes):
            pg.op("dve", lambda e: e.memset(out, val), [], writes)

        def dma(q, out, in_, reads, writes, semc):
            return pg.op(q, lambda e: e.dma_start(out=out, in_=in_), reads, writes, dma_sem=semc)

        def vcol(name, i):
            c = VEC[name] + i
            return VECS[:, c:c + 1]

        for dst, src in ((VECS, vecs_d), (MASKT, maskT_d), (QDEC, qdec_d), (KDEC, kdec_d), (IDF, ident_d),
                         (SGMASK, sgmask_d), (SGB, sgb_d), (HALOM, halom_d)):
            dma("sp", dst[:], src, [], [CONST], constsem)
        dma("sp", RG[:], o_router.rearrange("(c p) e -> p c e", p=P), [], [CONST], constsem)
        dma("sp", UT[:], ut_d, [], [CONST], constsem)
        dma("sp", EOFF[:], eoff_d, [], [CONST], constsem)
        memset(RUN[:], 0.0, [RUNb])
        dma("pool", IDB[:], ident_d, [], [CONST], pg.new_semc())
        memset(ONES[:], 1.0, [CONST])
        memset(EPSC[:], EPS, [CONST])
        memset(E0[:], 0.0, [CONST])
        memset(E0[0:1, :], 1.0, [CONST])
        memset(STATE[:], 0.0, [STATEb])
        memset(STATEB[:], 0.0, [STATEBb])
        memset(CARRY0[:], 0.0, [CARRY0b])
        memset(CARRY1[:], 0.0, [CARRY1b])
        dma("sp", CONVO[:, :, 0:P], sgwT_d, [], CONVOb, constsem)
        tt(SGW[:], CONVO[:, :, 0:P], SGMASK[:].unsqueeze(1).to_broadcast([P, 4, P]), ALU.mult,
           [CONST] + CONVOb, [CONST])
        for c in range(KC):
            ts(RG[:, c, :], RG[:, c, :], vcol("o_norm_ffn", c), None, ALU.mult, None, [CONST], [CONST])

        def load_x(slot, col0, W):
            dma("sp", Hs(slot, W), xT[:, :, col0:col0 + W], [], [Hbuf[slot]], Hsem[slot])

        def load_tabs(par, col0, W):
            dma("sp", COS[par][0][:, :W], cosT[:, col0:col0 + W], [], [COS[par][1]], tabsem[par][0])
            dma("sp", SIN[par][0][:, :W], sinT[:, col0:col0 + W], [], [SIN[par][1]], tabsem[par][1])

        def stats_rstd(srcs, W, scale, reads):
            ps, psb = aux_rot.next()
            n = len(srcs)
            for i, s in enumerate(srcs):
                sq, sqb = sq_rot.next()
                act(sq[:, :W], s, AF.Square, reads, [sqb])
                mm(ps[:, :W], ONES[:], sq[:, :W], i == 0, i == n - 1, [sqb, CONST], [psb])
            act(RSTD[:, :W], ps[:, :W], AF.Sqrt, [psb, CONST], [RSTDb], bias=EPSC[:], scale=scale)
            recip(RSTD[:, :W], RSTD[:, :W], [RSTDb], [RSTDb])

        def rmsnorm(slot, W, gname):
            h = Hs(slot, W)
            stats_rstd([h[:, c, :] for c in range(KC)], W, 1.0 / D, [Hbuf[slot]])
            hn = HNs(slot, W)
            for c in range(KC):
                stt(hn[:, c, :], h[:, c, :], vcol(gname, c), RSTD[:, :W], ALU.mult, ALU.mult,
                    [Hbuf[slot], RSTDb, CONST], [HNbuf[slot]])

        def ln_stats(srcs, srcbufs, W, n_feat):
            ps1, ps1b = aux_rot.next()
            ps2, ps2b = aux_rot.next()
            n = len(srcs)
            for i, (s, b) in enumerate(zip(srcs, srcbufs)):
                sq, sqb = sq_rot.next()
                act(sq[:, :W], s, AF.Square, [b], [sqb])
                mm(ps1[:, :W], ONES[:], s, i == 0, i == n - 1, [b, CONST], [ps1b])
                mm(ps2[:, :W], ONES[:], sq[:, :W], i == 0, i == n - 1, [sqb, CONST], [ps2b])
            tb, tbb = tb_rot.next()
            act(MEAN[:, :W], ps1[:, :W], AF.Copy, [ps1b], [MEANb], scale=1.0 / n_feat)
            tt(tb[:, :W], MEAN[:, :W], MEAN[:, :W], ALU.mult, [MEANb], [tbb])
            stt(VAR[:, :W], ps2[:, :W], 1.0 / n_feat, tb[:, :W], ALU.mult, ALU.subtract, [ps2b, tbb], [VARb])
            ts(VAR[:, :W], VAR[:, :W], 0.0, None, ALU.max, None, [VARb], [VARb])
            act(VAR[:, :W], VAR[:, :W], AF.Sqrt, [VARb, CONST], [VARb], bias=EPSC[:], scale=1.0)
            recip(VAR[:, :W], VAR[:, :W], [VARb], [VARb])

        def proj_chunk(wtile, wbuf, col, slot, W):
            ps, psb = mm_rot.next()
            hn = HNs(slot, W)
            for c in range(KC):
                mm(ps[:, :W], wtile[:, c, col:col + P], hn[:, c, :], c == 0, c == KC - 1,
                   [wbuf, HNbuf[slot]], [psb])
            return ps, psb

        def rotary(ps, psb, dst, W, par):
            ta, tab = ta_rot.next()
            tb, tbb = tb_rot.next()
            cos, cosb = COS[par]
            sin, sinb = SIN[par]
            tt(ta[:, :W], ps[:, :W], cos[:, :W], ALU.mult, [psb, cosb], [tab])
            tt(tb[0:64, :W], ps[64:128, :W], sin[64:128, :W], ALU.mult, [psb, sinb], [tbb])
            tt(tb[64:128, :W], ps[0:64, :W], sin[0:64, :W], ALU.mult, [psb, sinb], [tbb])
            return ta, tab, tb, tbb

        def Yv(slot):
            o = {0: 1, 1: 2, 2: 1}[slot]
            return HNB[:, :, slot_off[o]:slot_off[o] + TW], HNbuf[o]

        def wout_residual(wtile, wbuf, slot, W):
            Y, Yb = Yv(slot)
            h = Hs(slot, W)
            for oc in range(KC):
                ps, psb = dn_rot.next()
                for c in range(KC):
                    mm(ps[:, :W], wtile[:, c, oc * P:(oc + 1) * P], Y[:, c, :W], c == 0, c == KC - 1,
                       [wbuf, Yb], [psb])
                tt(h[:, oc, :], h[:, oc, :], ps[:, :W], ALU.add, [Hbuf[slot], psb], [Hbuf[slot]])

        def load_w(dst, dstbuf, semc, src, ncols):
            for c in range(KC):
                dma("pool", dst[:, c, :ncols], src[c * P:(c + 1) * P, :], [], [dstbuf], semc)

        BS0 = (ROTK, ROTKb, VT, VTb, KD, KDb)
        BS1 = (ROTQ, ROTQb, QD, QDb, SILUG, SILUGb)

        def mixer0_tile(slot, W, par, mode, stage="12B", bs=None):
            full = mode == "full"
            nblk = W // P
            Y, Yb = Yv(slot)
            WIN = WIN0
            RK, RKb, VTx, VTxb, KDx, KDxb = BS0 if bs is None else bs
            if "1" in stage:
                rmsnorm(slot, W, "e_norm_mix")
            if "2" in stage:
                mixer0_stageA(slot, W, par, mode, full, nblk, WIN, RK, RKb, VTx, VTxb)
            if "B" in stage:
                mixer0_stageB(slot, W, par, mode, full, nblk, WIN, Y, Yb, RK, RKb, VTx, VTxb, KDx, KDxb)

        def mixer0_stageA(slot, W, par, mode, full, nblk, WIN, RK, RKb, VTx, VTxb):
            for h in range(4):
                ps, psb = proj_chunk(WIN, WINb, 512 + h * P, slot, W)
                ta, tab, tb, tbb = rotary(ps, psb, None, W, par)
                tt(RK[:, h, :W], ta[:, :W], tb[:, :W], ALU.add, [tab, tbb], [RKb])
            if full:
                for h in range(4):
                    ps, psb = proj_chunk(WIN, WINb, h * P, slot, W)
                    ta, tab, tb, tbb = rotary(ps, psb, None, W, par)
                    tt(ROTQ[:, h, :W], ta[:, :W], tb[:, :W], ALU.add, [tab, tbb], [ROTQb])
                for h in range(4):
                    tt(QD[:, h, :W].rearrange("p (b c) -> p b c", c=P),
                       ROTQ[:, h, :W].rearrange("p (b c) -> p b c", c=P),
                       QDEC[:, h, :].unsqueeze(1).to_broadcast([P, nblk, P]), ALU.mult,
                       [ROTQb, CONST], [QDb])
            hn = HNs(slot, W)
            for b in range(nblk):
                ps, psb = mm_rot.next()
                for c in range(KC):
                    mm(ps[:, :], hn[:, c, b * P:(b + 1) * P], WIN[:, c, 1024:1536], c == 0, c == KC - 1,
                       [WINb, HNbuf[slot]], [psb])
                act(VTx[:, b, :], ps[:, :], AF.Copy, [psb], [VTxb])
            if full:
                for h in range(4):
                    ps, psb = proj_chunk(WIN, WINb, 1536 + h * P, slot, W)
                    act(SILUG[:, h, :W], ps[:, :W], AF.Silu, [psb], [SILUGb])
            if mode in ("full", "kvab"):
                vcopy(CBUF[:, :, 0:30], CARRY0[:], [CARRY0b], [CBUFb])
                for c in range(4):
                    psa, psab = proj_chunk(WIN, WINb, 2048 + c * P, slot, W)
                    psg, psgb = proj_chunk(WIN, WINb, 2560 + c * P, slot, W)
                    ta, tab = ta_rot.next()
                    act(ta[:, :W], psg[:, :W], AF.Sigmoid, [psgb], [tab])
                    tt(CBUF[:, c, 30:30 + W], psa[:, :W], ta[:, :W], ALU.mult, [psab, tab], [CBUFb])
                vcopy(CARRY0[:], CBUF[:, :, W:W + 30], [CBUFb], [CARRY0b])
        def mixer0_stageB(slot, W, par, mode, full, nblk, WIN, Y, Yb, RK, RKb, VTx, VTxb, KDx, KDxb):
            for b in range(nblk):
                ps, psb = aux_rot.next()
                psv = ps[:].bitcast(BF16)
                for h in range(4):
                    tr(psv[:, h * P:(h + 1) * P], RK[:, h, b * P:(b + 1) * P], [RKb], [psb])
                tt(KDx[:, b, :].rearrange("p (h d) -> p h d", d=P),
                   psv[:, 0:512].rearrange("p (h d) -> p h d", d=P),
                   KDEC[:].unsqueeze(2).to_broadcast([P, 4, P]), ALU.mult, [psb, CONST], [KDxb])
            for b in range(nblk):
                cols = slice(b * P, (b + 1) * P)
                if full:
                    ps, psb = aux_rot.next()
                    for h in range(4):
                        mm(ps[:, h * P:(h + 1) * P], RK[:, h, cols], ROTQ[:, h, cols], True, True,
                           [RKb, ROTQb], [psb])
                    pt, ptb = pt_rot.next()
                    tt(pt[:], ps[:].rearrange("p (h c) -> p h c", c=P), MASKT[:], ALU.mult, [psb, CONST], [ptb])
                    po, pob = acc_rot.next()
                    for h in range(4):
                        mm(po[:, h * P:(h + 1) * P], VTx[:, b, h * P:(h + 1) * P], pt[:, h, :], True, False,
                           [VTxb, ptb], [pob])
                        mm(po[:, h * P:(h + 1) * P], STATEB[:, h, :], QD[:, h, cols], False, True,
                           [STATEBb, QDb], [pob])
                    act(CONVO[:, :, cols], po[:].rearrange("p (h c) -> p h c", c=P), AF.Copy, [pob], CONVOb)
                pk, pkb = acc_rot.next()
                for h in range(4):
                    mm(pk[:, h * P:(h + 1) * P], KDx[:, b, h * P:(h + 1) * P], VTx[:, b, h * P:(h + 1) * P],
                       True, True, [KDxb, VTxb], [pkb])
                for h in range(4):
                    stt(STATE[:, h, :], STATE[:, h, :], cdec[h], pk[:, h * P:(h + 1) * P], ALU.mult, ALU.add,
                        [STATEb, pkb], [STATEb])
                act(STATEB[:], STATE[:], AF.Copy, [STATEb], [STATEBb])
            if not full:
                return
            for h in range(4):
                r = CONVO[:, h, :W]
                ln_stats([r], [CONVOb[h]], W, P)
                ta, tab = ta_rot.next()
                tt(ta[:, :W], r, MEAN[:, :W], ALU.subtract, [CONVOb[h], MEANb], [tab])
                tt(ta[:, :W], ta[:, :W], VAR[:, :W], ALU.mult, [tab, VARb], [tab])
                stt(Y[:, h, :W], ta[:, :W], vcol("e_ret_gn", h), SILUG[:, h, :W], ALU.mult, ALU.mult,
                    [tab, SILUGb, CONST], [Yb])
            for c in range(4):
                ps, psb = mm_rot.next()
                for j in range(CONV_K):
                    dg, dgb = dg_rot.next()
                    wc = VEC["e_dw_w"] + j * 4 + c
                    ts(dg[:, :], IDB[:], VECS[:, wc:wc + 1], None, ALU.mult, None, [CONST], [dgb])
                    mm(ps[:, :W], dg[:, :], CBUF[:, c, j:j + W], j == 0, j == CONV_K - 1, [dgb, CBUFb], [psb])
                ts(CONVO[:, c, :W], ps[:, :W], vcol("e_dw_b", c), None, ALU.add, None, [psb, CONST], [CONVOb[c]])
            ln_stats([CONVO[:, c, :W] for c in range(4)], CONVOb, W, 512)
            for c in range(4):
                ta, tab = ta_rot.next()
                tt(ta[:, :W], CONVO[:, c, :W], MEAN[:, :W], ALU.subtract, [CONVOb[c], MEANb], [tab])
                tt(ta[:, :W], ta[:, :W], VAR[:, :W], ALU.mult, [tab, VARb], [tab])
                act(Y[:, 4 + c, :W], ta[:, :W], AF.Silu, [tab, CONST], [Yb],
                    bias=vcol("e_cv_ln_b", c), scale=vcol("e_cv_ln_g", c))
            wout_residual(WOUT, WOUTb, slot, W)

        def gelu_from_psum(ps, psb, W, out, outreads, outwrites):
            sq, sqb = sq_rot.next()
            ta, tab = ta_rot.next()
            act(sq[:, :W], ps[:, :W], AF.Square, [psb], [sqb])
            ts(ta[:, :W], sq[:, :W], 0.044715, 1.0, ALU.mult, ALU.add, [sqb], [tab])
            tt(ta[:, :W], ta[:, :W], ps[:, :W], ALU.mult, [tab, psb], [tab])
            act(ta[:, :W], ta[:, :W], AF.Sigmoid, [tab], [tab], scale=GELU_C)
            tt(out, ps[:, :W], ta[:, :W], ALU.mult, [psb, tab] + outreads, outwrites)

        def mixer1_tile(slot, W, mode):
            full = mode == "full"
            nblk = W // P
            Y, Yb = Yv(slot)
            WIN = WIN1
            rmsnorm(slot, W, "o_norm_mix")
            if full:
                for c in range(4):
                    ps, psb = proj_chunk(WIN, WINb, c * P, slot, W)
                    gelu_from_psum(ps, psb, W, SILUG[:, c, :W], [], [SILUGb])
                for c in range(4):
                    ps, psb = proj_chunk(WIN, WINb, 512 + c * P, slot, W)
                    gelu_from_psum(ps, psb, W, CONVO[:, c, :W], [], [CONVOb[c]])
                ln_stats([CONVO[:, c, :W] for c in range(4)], CONVOb, W, 512)
                for c in range(4):
                    ta, tab = ta_rot.next()
                    tt(ta[:, :W], CONVO[:, c, :W], MEAN[:, :W], ALU.subtract, [CONVOb[c], MEANb], [tab])
                    tt(ta[:, :W], ta[:, :W], VAR[:, :W], ALU.mult, [tab, VARb], [tab])
                    act(ROTQ[:, c, :W], ta[:, :W], AF.Identity, [tab, CONST], [ROTQb],
                        bias=vcol("o_sg_ln_b", c), scale=vcol("o_sg_ln_g", c))
                for b in range(nblk):
                    ps, psb = aux_rot.next()
                    psv = ps[:].bitcast(BF16)
                    for g in range(4):
                        tr(psv[:, g * P:(g + 1) * P], ROTQ[:, g, b * P:(b + 1) * P], [ROTQb], [psb])
                    vcopy(VT[:, b, :], psv[:, 0:512], [psb], [VTb])
                for b in range(nblk):
                    cols = slice(b * P, (b + 1) * P)
                    ps, psb = acc_rot.next()
                    for g in range(4):
                        mm(ps[:, g * P:(g + 1) * P], VT[:, b, g * P:(g + 1) * P], SGW[:, g, :], True, True,
                           [VTb, CONST], [psb])
                    ta, tab = ta_rot.next()
                    tav = ta[:].rearrange("p (g c) -> p g c", c=P)
                    tt(tav, ps[:].rearrange("p (g c) -> p g c", c=P), SGB[:], ALU.add, [psb, CONST], [tab])
                    tt(Y[:, 0:4, cols], tav, SILUG[:, :, cols], ALU.mult, [tab, SILUGb], [Yb])
            vcopy(MBUF[:, :, 0:2], CARRY1[:], [CARRY1b], MBb)
            for c in range(4):
                psc, pscb = proj_chunk(WIN, WINb, 1536 + c * P, slot, W)
                psh, pshb = proj_chunk(WIN, WINb, 2048 + c * P, slot, W)
                ta, tab = ta_rot.next()
                act(ta[:, :W], psc[:, :W], AF.Copy, [pscb], [tab])
                tt(MBUF[:, c, 2:2 + W], psh[:, :W], ta[:, :W], ALU.mult, [pshb, tab], MBb)
                if full:
                    psg, psgb = proj_chunk(WIN, WINb, 1024 + c * P, slot, W)
                    tb, tbb = tb_rot.next()
                    w0 = VEC["o_sc_w"] + c
                    ts(tb[:, :W], MBUF[:, c, 0:W], VECS[:, w0:w0 + 1], None, ALU.mult, None, MBb + [CONST], [tbb])
                    stt(tb[:, :W], MBUF[:, c, 1:1 + W], VECS[:, w0 + 4:w0 + 5], tb[:, :W], ALU.mult, ALU.add,
                        MBb + [tbb, CONST], [tbb])
                    stt(tb[:, :W], MBUF[:, c, 2:2 + W], VECS[:, w0 + 8:w0 + 9], tb[:, :W], ALU.mult, ALU.add,
                        MBb + [tbb, CONST], [tbb])
                    tt(Y[:, 4 + c, :W], tb[:, :W], psg[:, :W], ALU.mult, [tbb, psgb], [Yb])
            if full:
                vcopy(CARRY1[:], MBUF[:, :, W:W + 2], MBb, [CARRY1b])
                wout_residual(WOUT, WOUTb, slot, W)
            else:
                ts(CARRY1[:], MBUF[:, :, W:W + 2], HALOM[:, 0:1], None, ALU.mult, None, MBb + [CONST], [CARRY1b])

        def load_group(fs, wg, wu, wd, f0):
            for c in range(KC):
                dma("pool", fs["g"][:, c, :], wg[c * P:(c + 1) * P, f0:f0 + G * P], [], [fs["buf"]], fs["sem"])
            for c in range(KC):
                dma("pool", fs["u"][:, c, :], wu[c * P:(c + 1) * P, f0:f0 + G * P], [], [fs["buf"]], fs["sem"])
            for j in range(G):
                dma("pool", fs["d"][:, j, :], wd[f0 + j * P:f0 + (j + 1) * P, :], [], [fs["buf"]], fs["sem"])

        def ffn_group_compute(fs, slot, W, gw=None):
            hn = HNs(slot, W)
            h = Hs(slot, W)
            ab, abb = ab_rot.next()
            for j in range(G):
                pg_, pgb = mm_rot.next()
                pu_, pub = mm_rot.next()
                for c in range(KC):
                    mm(pg_[:, :W], fs["g"][:, c, j * P:(j + 1) * P], hn[:, c, :], c == 0, c == KC - 1,
                       [fs["buf"], HNbuf[slot]], [pgb])
                for c in range(KC):
                    mm(pu_[:, :W], fs["u"][:, c, j * P:(j + 1) * P], hn[:, c, :], c == 0, c == KC - 1,
                       [fs["buf"], HNbuf[slot]], [pub])
                if gw is None:
                    sg, sgb_ = sgt_rot.next()
                    act(sg[:, :W], pg_[:, :W], AF.Silu, [pgb], [sgb_])
                    tt(ab[:, j, :W], pu_[:, :W], sg[:, :W], ALU.mult, [pub, sgb_], [abb])
                else:
                    gwt, gwb, gcol = gw
                    ta, tab = ta_rot.next()
                    act(ta[:, :W], pg_[:, :W], AF.Silu, [pgb], [tab])
                    tt(ta[:, :W], pu_[:, :W], ta[:, :W], ALU.mult, [pub, tab], [tab])
                    tt(ab[:, j, :W], ta[:, :W], gwt[:, gcol:gcol + W], ALU.mult, [tab, gwb], [abb])
            for oc in range(KC):
                ps, psb = dn_rot.next()
                for j in range(G):
                    mm(ps[:, :W], fs["d"][:, j, oc * P:(oc + 1) * P], ab[:, j, :W], j == 0, j == G - 1,
                       [fs["buf"], abb], [psb])
                tt(h[:, oc, :], h[:, oc, :], ps[:, :W], ALU.add, [Hbuf[slot], psb], [Hbuf[slot]])

        _breg = {}

        def bound_reg(e):
            if "r" not in _breg:
                _breg["r"] = e.to_reg(NSLOT - 1)
            return _breg["r"]

        def router_tile(slot, W, blk0):
            h = Hs(slot, W)
            for b in range(W // P):
                cols = slice(b * P, (b + 1) * P)
                ps, psb = aux_rot.next()
                for c in range(KC):
                    mm(ps[:, 0:NEXP], h[:, c, cols], RG[:, c, :], c == 0, c == KC - 1, [Hbuf[slot], CONST], [psb])
                mm(ps[:, 8:9], RSTD[:, cols], E0[:], True, True, [RSTDb, CONST], [psb])
                act(LG[:, 0:9], ps[:, 0:9], AF.Copy, [psb], [LGb])
                L, Lb = RT["L"]
                MK1, MK1b = RT["MK1"]
                L2, L2b = RT["L2"]
                MK2, MK2b = RT["MK2"]
                M1, M1b = RS["M1"]
                M2, M2b = RS["M2"]
                Dd, Db = RS["D"]
                Ee, Eb = RS["E"]
                W1, W1b = RS["W1"]
                W2, W2b = RS["W2"]
                ts(L[:], LG[:, 0:8], LG[:, 8:9], None, ALU.mult, None, [LGb], [Lb])
                pg.op("dve", lambda e: e.tensor_reduce(M1[:], L[:], mybir.AxisListType.X, ALU.max), [Lb], [M1b])
                ts(MK1[:], L[:], M1[:, 0:1], None, ALU.is_equal, None, [Lb, M1b], [MK1b])
                stt(L2[:], MK1[:], -1e30, L[:], ALU.mult, ALU.add, [MK1b, Lb], [L2b])
                pg.op("dve", lambda e: e.tensor_reduce(M2[:], L2[:], mybir.AxisListType.X, ALU.max), [L2b], [M2b])
                ts(MK2[:], L2[:], M2[:, 0:1], None, ALU.is_equal, None, [L2b, M2b], [MK2b])
                tt(Dd[:], M2[:], M1[:], ALU.subtract, [M1b, M2b], [Db])
                act(Ee[:], Dd[:], AF.Exp, [Db], [Eb])
                ts(W1[:], Ee[:], 1.0, None, ALU.add, None, [Eb], [W1b])
                recip(W1[:], W1[:], [W1b], [W1b])
                tt(W2[:], Ee[:], W1[:], ALU.mult, [Eb, W1b], [W2b])
                blkg = blk0 + b
                MS, MSb = RT["G"]
                POS, POSb = RT["POS"]
                OK8, OK8b = RT["OK"]
                SL, SLb = RT["SL"]
                T8, T8b = RT["T8"]
                SA, SAb = RS["SA"]
                SBs, SBsb = RS["SB"]
                tt(MS[:], MK1[:], MK2[:], ALU.add, [MK1b, MK2b], [MSb])
                ps2, ps2b = aux_rot.next()
                mm(ps2[:, 0:NEXP], UT[:], MS[:], True, True, [CONST, MSb], [ps2b])
                mm(ps2[:, 8:16], ONES[:], MS[:], True, True, [CONST, MSb], [ps2b])
                tt(POS[:], ps2[:, 0:NEXP], RUN[:], ALU.add, [ps2b, RUNb], [POSb])
                tt(RUN[:], RUN[:], ps2[:, 8:16], ALU.add, [ps2b, RUNb], [RUNb])
                ts(OK8[:], POS[:], float(CAP), None, ALU.is_lt, None, [POSb], [OK8b])
                tt(SL[:], POS[:], EOFF[:], ALU.add, [POSb, CONST], [SLb])
                stt(SL[:], SL[:], float(-ZROW), OK8[:], ALU.add, ALU.mult, [SLb, OK8b], [SLb])
                ts(SL[:], SL[:], float(ZROW), None, ALU.add, None, [SLb], [SLb])
                tt(T8[:], MK1[:], SL[:], ALU.mult, [MK1b, SLb], [T8b])
                pg.op("dve", lambda e: e.tensor_reduce(SA[:], T8[:], mybir.AxisListType.X, ALU.add), [T8b], [SAb])
                vcopy(IDXA[:, blkg:blkg + 1], SA[:], [SAb], [IDXAb])
                tt(T8[:], MK2[:], SL[:], ALU.mult, [MK2b, SLb], [T8b])
                pg.op("dve", lambda e: e.tensor_reduce(SBs[:], T8[:], mybir.AxisListType.X, ALU.add), [T8b], [SBsb])
                vcopy(IDXB[:, blkg:blkg + 1], SBs[:], [SBsb], [IDXBb])
                vcopy(WA[:, blkg:blkg + 1], W1[:], [W1b], [WAb])
                vcopy(WB[:, blkg:blkg + 1], W2[:], [W2b], [WBb])
                hn = HNs(slot, W)
                pt_, ptb_ = aux_rot.next()
                ptv = pt_[:].bitcast(BF16)
                for c in range(KC):
                    tr(ptv[:, c * P:(c + 1) * P], hn[:, c, cols], [HNbuf[slot]], [ptb_])
                hnt, hntb = hnt_rot.next()
                act(hnt[:, :], ptv[:, 0:D], AF.Copy, [ptb_], [hntb])
                for idx, idxb in ((IDXA, IDXAb), (IDXB, IDXBb)):
                    iap = idx[:, blkg:blkg + 1]
                    pg.op("pool", (lambda iap, hnt: lambda e: e.indirect_dma_start(
                        out=XG, out_offset=bass.IndirectOffsetOnAxis(iap, 0), in_=hnt[:, :], in_offset=None,
                        bounds_check=bound_reg(e), oob_is_err=False))(iap, hnt), [hntb, idxb], [XGb], dma_sem=xg_sem)
                pa, pab = mm_rot.next()
                pb, pbb = mm_rot.next()
                for c in range(KC):
                    pp, ppb = (pa, pab) if c < 4 else (pb, pbb)
                    mm(pp[:, (c % 4) * P:(c % 4 + 1) * P], h[:, c, cols], IDF[:], True, True, [Hbuf[slot], CONST], [ppb])
                h3s, h3sb = h3s_rot.next()
                act(h3s[:, 0:512], pa[:, :], AF.Copy, [pab], [h3sb])
                vcopy(h3s[:, 512:1024], pb[:, :], [pbb], [h3sb])
                dma("sp", H3D[blkg * P:(blkg + 1) * P, :], h3s[:, :], [h3sb], [H3Db], h3d_sem)

        n_pre = PRE // TW
        out_sem = pg.new_semc()
        out_ops = []
        tab_i = [0]

        def next_par():
            p = tab_i[0] % 2
            tab_i[0] += 1
            return p

        for st in range(NST):
            main_slots = list(range(1, 1 + ST_TILES))
            main_col0 = [PRE + HALO + (st * ST_TILES + i) * TW for i in range(ST_TILES)]
            fence([], [WINb] + FFN_BUFS)
            load_w(WIN0, WINb, WINsem, e_w_in, 3072)
            load_w(WOUT, WOUTb, WOUTsem, e_w_out, D)
            seq = []
            if st == 0:
                for i in range(n_pre):
                    seq.append((1 + (i % ST_TILES), i * TW, TW, "kvab" if i == n_pre - 1 else "kv"))
                seq.append((0, PRE, HALO, "full"))
            for i in range(ST_TILES):
                seq.append((main_slots[i], main_col0[i], TW, "full"))
            pars = [next_par() for _ in seq]
            done_x, done_t = set(), set()

            def ensure_x(i):
                if i < len(seq) and i not in done_x:
                    done_x.add(i)
                    load_x(seq[i][0], seq[i][1], seq[i][2])

            def ensure_t(i):
                if i < len(seq) and i not in done_t:
                    done_t.add(i)
                    load_tabs(pars[i], seq[i][1], seq[i][2])

            npf = sum(1 for t in seq if t[3] != "full")

            def targs(i):
                s_, c0_, W_, m_ = seq[i]
                return (s_, W_, pars[i], m_)

            for i in range(min(2, len(seq))):
                ensure_x(i)
                ensure_t(i)
            if npf > 0:
                mixer0_tile(*targs(0), stage="1", bs=BS0)
                ensure_x(2)
                for k in range(npf):
                    bs = BS0 if k % 2 == 0 else BS1
                    if k + 1 < npf:
                        mixer0_tile(*targs(k + 1), stage="1", bs=(BS0 if (k + 1) % 2 == 0 else BS1))
                        ensure_x(k + 3)
                    mixer0_tile(*targs(k), stage="2", bs=bs)
                    ensure_t(k + 2)
                    if k >= 1:
                        mixer0_tile(*targs(k - 1), stage="B", bs=(BS0 if (k - 1) % 2 == 0 else BS1))
                mixer0_tile(*targs(npf - 1), stage="B", bs=(BS0 if (npf - 1) % 2 == 0 else BS1))
            for k in range(npf, len(seq)):
                ensure_x(k)
                ensure_t(k)
                mixer0_tile(*targs(k))
                ensure_x(k + 1)
                ensure_t(k + 1)
                ensure_x(k + 2)
                ensure_t(k + 2)
            tiles = ([(0, HALO)] if st == 0 else []) + [(s, TW) for s in main_slots]
            ngrp = D_FF // (G * P)
            fence([], [WINb] + FFN_BUFS)
            fsl = [fs_rot.next() for _ in range(ngrp)]
            load_group(fsl[0], e_ffn_gate, e_ffn_up, e_ffn_down, 0)
            for (s, W) in tiles:
                rmsnorm(s, W, "e_norm_ffn")
            for gi in range(ngrp):
                if gi + 1 < ngrp:
                    load_group(fsl[gi + 1], e_ffn_gate, e_ffn_up, e_ffn_down, (gi + 1) * G * P)
                for (s, W) in tiles:
                    ffn_group_compute(fsl[gi], s, W)
            fence([], [WINb] + FFN_BUFS)
            load_w(WIN1, WINb, WINsem, o_w_in, 2560)
            load_w(WOUT, WOUTb, WOUTsem, o_w_out, D)
            for (s, W) in tiles:
                mixer1_tile(s, W, "carry" if s == 0 else "full")
            fence([], [WINb] + FFN_BUFS)
            for ti, (s, W) in enumerate([(s_, TW) for s_ in main_slots]):
                rmsnorm(s, W, "o_norm_ffn")
                router_tile(s, W, (st * ST_TILES + ti) * (TW // P))

        _ao[0] = 0
        FNG, FNGb = atmp("FNG", [P, D], F32, P2_BUFS)
        _p2base = _ao[0]
        YTM = ar([P, CB, D], F32)
        YTMb = [Buf("YTM%d" % i) for i in range(CB)]
        XT = ar([P, KC, CAP], BF16)
        XTb = Buf("XT")
        XGT = [atmp("XGT%d" % i, [P, D], BF16, P2_BUFS) for i in range(2)]
        xgt_sem = [pg.new_semc() for _ in range(2)]
        NFIN = 8
        fin_sem = [pg.new_semc() for _ in range(NFIN)]
        ya_sem = [pg.new_semc() for _ in range(NFIN)]
        yb_sem = [pg.new_semc() for _ in range(NFIN)]
        fng_sem = pg.new_semc()
        P2_BUFS.extend(YTMb + [XTb])
        fence([], P1_BUFS + P2_BUFS + [WINb] + FFN_BUFS)
        dma("sp", FNG[:, :], fng_d, [], [FNGb], fng_sem)
        zt, ztb = ta_rot.next()
        memset(zt[0:1, :], 0.0, [ztb])
        dma("sp", YS[ZROW:ZROW + 1, 0:TW], zt[0:1, :], [ztb], [YSb], ys_sem)
        dma("sp", YS[ZROW:ZROW + 1, TW:D], zt[0:1, :], [ztb], [YSb], ys_sem)

        slot_tiles = []
        n0 = 0
        while n0 < CAP:
            w = min(TW, CAP - n0)
            slot_tiles.append((n0, w))
            n0 += w
        ngrp = D_EXP // (G * P)
        allg = [(e, gi) for e in range(NEXP) for gi in range(ngrp)]
        fsl = [fs_rot.next() for _ in allg]
        load_group(fsl[0], o_exp_gate[0], o_exp_up[0], o_exp_down[0], 0)
        xi = 0
        for k, (e, gi) in enumerate(allg):
            if k + 1 < len(allg):
                e2, g2 = allg[k + 1]
                load_group(fsl[k + 1], o_exp_gate[e2], o_exp_up[e2], o_exp_down[e2], g2 * G * P)
            fs = fsl[k]
            if gi == 0:
                for b in range(CB):
                    par = xi % 2
                    xi += 1
                    xgt, xgtb = XGT[par]
                    r0 = e * CAP + b * P
                    dma("sp", xgt[:, :], XG[r0:r0 + P, :], [XGb], [xgtb], xgt_sem[par])
                    pt_, ptb_ = aux_rot.next()
                    ptv = pt_[:].bitcast(BF16)
                    for c in range(KC):
                        tr(ptv[:, c * P:(c + 1) * P], xgt[:, c * P:(c + 1) * P], [xgtb], [ptb_])
                    act(XT[:, :, b * P:(b + 1) * P], ptv[:, 0:D].rearrange("p (c s) -> p c s", c=KC), AF.Copy,
                        [ptb_], [XTb])
            for (n0, W) in slot_tiles:
                ab, abb = ab_rot.next()
                for j in range(G):
                    pg_, pgb = mm_rot.next()
                    pu_, pub = mm_rot.next()
                    for c in range(KC):
                        mm(pg_[:, :W], fs["g"][:, c, j * P:(j + 1) * P], XT[:, c, n0:n0 + W], c == 0, c == KC - 1,
                           [fs["buf"], XTb], [pgb])
                    for c in range(KC):
                        mm(pu_[:, :W], fs["u"][:, c, j * P:(j + 1) * P], XT[:, c, n0:n0 + W], c == 0, c == KC - 1,
                           [fs["buf"], XTb], [pub])
                    sg, sgb_ = sgt_rot.next()
                    act(sg[:, :W], pg_[:, :W], AF.Silu, [pgb], [sgb_])
                    tt(ab[:, j, :W], pu_[:, :W], sg[:, :W], ALU.mult, [pub, sgb_], [abb])
                for sbk in range(W // P):
                    blk = n0 // P + sbk
                    for dh in range(2):
                        ps, psb = dn_rot.next()
                        for j in range(G):
                            mm(ps[:, :], ab[:, j, sbk * P:(sbk + 1) * P], fs["d"][:, j, dh * TW:(dh + 1) * TW],
                               j == 0, j == G - 1, [fs["buf"], abb], [psb])
                        ydst = YTM[:, blk, dh * TW:(dh + 1) * TW]
                        if gi == 0:
                            vcopy(ydst, ps[:, :], [psb], [YTMb[blk]])
                        else:
                            tt(ydst, ydst, ps[:, :], ALU.add, [YTMb[blk], psb], [YTMb[blk]])
            if gi == ngrp - 1:
                for b in range(CB):
                    r0 = e * CAP + b * P
                    dma("sp", YS[r0:r0 + P, :], YTM[:, b, :], [YTMb[b]], [YSb], ys_sem)

        SS, SSb = RS["SS"]
        assert _p2base + 3 * NFIN * 2 * D <= ARENA_N
        fviews = [ARENA[:, _p2base + i * 2 * D:_p2base + (i + 1) * 2 * D].bitcast(F32) for i in range(3 * NFIN)]
        H3T = [(fviews[3 * i], Buf("H3T%d" % i)) for i in range(NFIN)]
        YA = [(fviews[3 * i + 1], Buf("YA%d" % i)) for i in range(NFIN)]
        YB = [(fviews[3 * i + 2], Buf("YB%d" % i)) for i in range(NFIN)]
        fence([], YTMb + [XTb] + [b_ for _, b_ in XGT] + [b_ for _, b_ in H3T + YA + YB])
        for j in range(NBLKC):
            par = j % NFIN
            h3t, h3tb = H3T[par]
            ya, yab = YA[par]
            yb, ybb = YB[par]
            dma("sp", h3t[:, :], H3D[j * P:(j + 1) * P, :], [H3Db], [h3tb], fin_sem[par])
            for (yt, ytb, idx, idxb, ysm) in ((ya, yab, IDXA, IDXAb, ya_sem[par]), (yb, ybb, IDXB, IDXBb, yb_sem[par])):
                iap = idx[:, j:j + 1]
                pg.op("pool", (lambda iap, yt: lambda e: e.indirect_dma_start(
                    out=yt[:, :], out_offset=None, in_=YS, in_offset=bass.IndirectOffsetOnAxis(iap, 0)))(iap, yt),
                    [YSb, idxb], [ytb], dma_sem=ysm)
            stt(h3t[:, :], ya[:, :], WA[:, j:j + 1], h3t[:, :], ALU.mult, ALU.add, [yab, WAb, h3tb], [h3tb])
            stt(h3t[:, :], yb[:, :], WB[:, j:j + 1], h3t[:, :], ALU.mult, ALU.add, [ybb, WBb, h3tb], [h3tb])
            memset(SS[:], 0.0, [SSb])
            pg.op("act", (lambda ya, h3t: lambda e: e.activation(ya[:, :], h3t[:, :], AF.Square, accum_out=SS[:, 0:1]))(ya, h3t),
                  [h3tb, SSb], [yab, SSb])
            act(SS[:], SS[:], AF.Sqrt, [SSb, CONST], [SSb], bias=EPSC[:], scale=1.0 / D)
            recip(SS[:], SS[:], [SSb], [SSb])
            stt(h3t[:, :], h3t[:, :], SS[:, 0:1], FNG[:, :], ALU.mult, ALU.mult, [h3tb, SSb, FNGb], [h3tb])
            out_ops.append(dma("sp", out_d[j * P:(j + 1) * P, :], h3t[:, :], [h3tb], [], out_sem))
        if dbg:
            out_ops.append(dma("sp", dbg_idx[:, 0, :], IDXA[:], [IDXAb], [], out_sem))
            out_ops.append(dma("sp", dbg_idx[:, 1, :], IDXB[:], [IDXBb], [], out_sem))
            out_ops.append(dma("sp", dbg_w[:, 0, :], WA[:], [WAb], [], out_sem))
            out_ops.append(dma("sp", dbg_w[:, 1, :], WB[:], [WBb], [], out_sem))
        pg.op("sp", lambda e: e.wait_ge(out_sem.sem, out_sem.count), [], [])
        pg.emit()
    return nc


def _vec_cols(v):
    return np.ascontiguousarray(v.reshape(-1, P).T)


def host_inputs(inputs, PRE, MAIN, CAP, n_seq_cores, batch):
    x = np.asarray(inputs["x"], np.float32)
    B, S, _ = x.shape
    NTOK = PRE + HALO + MAIN
    g = lambda k: np.asarray(inputs[k], np.float32)
    vecs = np.zeros((P, NVEC), np.float32)

    def put(name, arr):
        a = _vec_cols(arr)
        vecs[:, VEC[name]:VEC[name] + a.shape[1]] = a

    put("e_norm_mix", g("e_norm_mix")[0]); put("e_ret_gn", g("e_ret_gn")[0]); put("e_dw_b", g("e_dw_b")[0])
    put("e_cv_ln_g", g("e_cv_ln_g")[0]); put("e_cv_ln_b", g("e_cv_ln_b")[0]); put("e_norm_ffn", g("e_norm_ffn")[0])
    put("o_norm_mix", g("o_norm_mix")[0]); put("o_sg_ln_g", g("o_sg_ln_g")[0]); put("o_sg_ln_b", g("o_sg_ln_b")[0])
    put("o_norm_ffn", g("o_norm_ffn")[0]); put("final_norm", g("final_norm"))
    dw = g("e_dw_w")[0]
    vecs[:, VEC["e_dw_w"]:VEC["e_dw_w"] + 124] = dw.reshape(31, 4, P).transpose(2, 0, 1).reshape(P, 124)
    sc = g("o_sc_w")[0]
    vecs[:, VEC["o_sc_w"]:VEC["o_sc_w"] + 12] = sc.reshape(3, 4, P).transpose(2, 0, 1).reshape(P, 12)

    scale = np.float32(128.0 ** -0.5)
    idx = np.arange(P)
    maskT = np.zeros((P, 4, P), np.float32)
    qdec = np.zeros((P, 4, P), np.float32)
    kdec = np.zeros((P, 4), np.float32)
    for h in range(4):
        gam = 1.0 - 2.0 ** (-5 - h)
        l = idx[:, None]; c = idx[None, :]
        m = (gam ** np.abs(c - l)) * ((l // 64) <= (c // 64)) * scale
        maskT[:, h, :] = m
        qdec[:, h, :] = (scale * gam ** (idx + 1.0))[None, :]
        kdec[:, h] = gam ** (127.0 - idx)
    ident = np.eye(P, dtype=np.float32)
    pch = idx // 64
    sgmask = (pch[None, :] >= pch[:, None]).astype(np.float32)
    sgwT = np.ascontiguousarray(g("o_sg_w")[0].transpose(2, 0, 1))
    sgb = np.ascontiguousarray(np.broadcast_to(g("o_sg_b")[0][None], (P, 4, P)))
    half = 64
    freqs = (10000.0 ** (-np.arange(half, dtype=np.float32) / half)).astype(np.float32)

    fng = np.ascontiguousarray(np.broadcast_to(g("final_norm")[None, :], (P, D)))
    ut = (idx[:, None] < idx[None, :]).astype(np.float32)
    eoff = np.ascontiguousarray(np.broadcast_to((np.arange(NEXP, dtype=np.float32) * CAP)[None, :], (P, NEXP)))
    shared = dict(fng=fng, ut=ut, eoff=eoff, vecs=vecs, maskT=maskT, qdec=qdec, kdec=kdec, ident=ident, sgmask=sgmask, sgwT=sgwT, sgb=sgb,
                  e_w_in=g("e_w_in")[0], e_w_out=g("e_w_out")[0], e_ffn_gate=g("e_ffn_gate")[0],
                  e_ffn_up=g("e_ffn_up")[0], e_ffn_down=g("e_ffn_down")[0], o_w_in=g("o_w_in")[0],
                  o_w_out=g("o_w_out")[0], o_router=g("o_router")[0], o_exp_gate=g("o_exp_gate")[0],
                  o_exp_up=g("o_exp_up")[0], o_exp_down=g("o_exp_down")[0])
    maps = []
    for b in range(batch):
        for q in range(n_seq_cores):
            s0 = q * MAIN
            start = s0 - PRE - HALO
            xt = np.zeros((NTOK, D), np.float32)
            lo = max(start, 0)
            xt[lo - start:, :] = x[b, lo:s0 + MAIN, :]
            xTc = np.ascontiguousarray(xt.T.reshape(KC, P, NTOK).transpose(1, 0, 2))
            pos = (np.arange(NTOK) + start).astype(np.float32)
            ang = (pos[None, :] * freqs[:, None]).astype(np.float32)
            cs = np.cos(ang).astype(np.float32); sn = np.sin(ang).astype(np.float32)
            cosT = np.concatenate([cs, cs], 0)
            sinT = np.concatenate([sn, -sn], 0)
            halom = np.full((P, 1), 0.0 if q == 0 else 1.0, np.float32)
            m = dict(shared)
            m.update(xT=xTc, cosT=np.ascontiguousarray(cosT), sinT=np.ascontiguousarray(sinT), halom=halom)
            maps.append(m)
    return maps


_NC_CACHE = {}


DBG = False
LAST = {}


def run(inputs, PRE, MAIN, CAP, n_seq_cores, batch):
    key = (PRE, MAIN, CAP, DBG)
    if key not in _NC_CACHE:
        _NC_CACHE[key] = build_program(PRE, MAIN, CAP, dbg=DBG)
    nc = _NC_CACHE[key]
    maps = host_inputs(inputs, PRE, MAIN, CAP, n_seq_cores, batch)
    n = len(maps)
    res = run_bass_kernel_spmd(nc, maps, core_ids=list(range(n)))
    S = n_seq_cores * MAIN
    if DBG:
        LAST["res"] = res.results
    out = np.zeros((batch, S, D), np.float32)
    i = 0
    for b in range(batch):
        for q in range(n_seq_cores):
            out[b, q * MAIN:(q + 1) * MAIN, :] = np.asarray(res.results[i]["out"])
            i += 1
    return out


def kernel(**inputs):
    return run(inputs, PRE=12288, MAIN=4096, CAP=1536, n_seq_cores=4, batch=2)
```

```python
import numpy as np
import concourse.bass as bass
import concourse.mybir as mybir
from concourse.bass_utils import run_bass_kernel_spmd
from contextlib import ExitStack

F32 = mybir.dt.float32
BF16 = mybir.dt.bfloat16
AF = mybir.ActivationFunctionType
ALU = mybir.AluOpType

P = 128
KC = 8
D = 1024
TW = 512
HALO = 128
ST_TILES = 2
D_FF = 2816
D_EXP = 3584
NEXP = 8
EPS = 1e-6
CONV_K = 31
GELU_C = 1.5957691216057308

VEC = {}
_o = 0
for _n, _w in (("e_norm_mix", 8), ("e_ret_gn", 4), ("e_dw_b", 4), ("e_cv_ln_g", 4), ("e_cv_ln_b", 4),
               ("e_norm_ffn", 8), ("o_norm_mix", 8), ("o_sg_ln_g", 4), ("o_sg_ln_b", 4),
               ("o_norm_ffn", 8), ("final_norm", 8), ("e_dw_w", 124), ("o_sc_w", 12)):
    VEC[_n] = _o
    _o += _w
NVEC = _o


class Buf:
    def __init__(self, name):
        self.name = name
        self.writers = {}
        self.dma_writers = []
        self.readers = {}
        self.dma_readers = []


class SemCounter:
    def __init__(self, sem):
        self.sem = sem
        self.count = 0


class Op:
    __slots__ = ("eng", "fn", "deps", "is_dma", "semc", "semval", "milestone", "idx")

    def __init__(self, eng, fn, is_dma=False, semc=None):
        self.eng = eng
        self.fn = fn
        self.deps = []
        self.is_dma = is_dma
        self.semc = semc
        self.semval = None
        self.milestone = False
        self.idx = None


COMPUTE = ("pe", "act", "dve")
ENGS = ("pe", "act", "dve", "pool", "sp")


class Prog:
    def __init__(self, nc, es):
        self.nc = nc
        self.es = es
        self.ops = {e: [] for e in ENGS}
        self.nsem = 0

    def new_semc(self):
        self.nsem += 1
        return SemCounter(self.es.enter_context(self.nc.semaphore("s%d" % self.nsem)))

    def op(self, eng, fn, reads=(), writes=(), dma_sem=None):
        is_dma = dma_sem is not None
        o = Op(eng, fn, is_dma, dma_sem)
        deps = []
        for b in reads:
            for e, w in b.writers.items():
                deps.append((w, "raw"))
            for w in b.dma_writers:
                deps.append((w, "raw"))
        for b in writes:
            for e, w in b.writers.items():
                deps.append((w, "waw"))
            for w in b.dma_writers:
                deps.append((w, "waw"))
            for e, r in b.readers.items():
                deps.append((r, "war"))
            for r in b.dma_readers:
                deps.append((r, "war"))
        seen = set()
        for d, kind in deps:
            if d is o or id(d) in seen:
                continue
            if (not d.is_dma) and (not is_dma) and d.eng == eng:
                if kind != "raw" or eng == "pe":
                    continue
            if d.is_dma and is_dma and d.semc is dma_sem and kind == "waw":
                continue
            seen.add(id(d))
            o.deps.append(d)
            d.milestone = True
        for b in writes:
            b.writers = {}
            b.dma_writers = []
            b.readers = {}
            b.dma_readers = []
            if is_dma:
                b.dma_writers.append(o)
            else:
                b.writers[eng] = o
        for b in reads:
            if b in writes:
                continue
            if is_dma:
                b.dma_readers.append(o)
            else:
                b.readers[eng] = o
        if is_dma:
            dma_sem.count += 16
            o.semval = dma_sem.count
        self.ops[eng].append(o)
        return o

    def emit(self):
        nc = self.nc
        EPOCH = 16000
        esems = {}
        for e in COMPUTE:
            n = 0
            for o in self.ops[e]:
                if o.milestone:
                    ep = n // EPOCH
                    if (e, ep) not in esems:
                        esems[(e, ep)] = self.es.enter_context(nc.semaphore("eng_%s_%d" % (e, ep)))
                    o.semc = esems[(e, ep)]
                    n += 1
                    o.semval = n - ep * EPOCH
            print("engine", e, "ops", len(self.ops[e]), "milestones", n)
        print("pool ops", len(self.ops["pool"]), "sp ops", len(self.ops["sp"]), "sems", self.nsem + len(esems))
        block = self.es.enter_context(nc.Block())

        def run(engname, eobj):
            waited = {}
            for o in self.ops[engname]:
                need = {}
                for d in o.deps:
                    sem = d.semc.sem if d.is_dma else d.semc
                    k = id(sem)
                    if k not in need or need[k][1] < d.semval:
                        need[k] = (sem, d.semval)
                for k, (sem, val) in need.items():
                    if waited.get(k, 0) < val:
                        eobj.wait_ge(sem, val)
                        waited[k] = val
                ins = o.fn(eobj)
                if o.is_dma:
                    ins.then_inc(o.semc.sem, 16)
                elif o.milestone:
                    ins.then_inc(o.semc, 1)

        @block.tensor
        def _(e):
            run("pe", e)

        @block.scalar
        def _(e):
            run("act", e)

        @block.vector
        def _(e):
            run("dve", e)

        @block.gpsimd
        def _(e):
            run("pool", e)

        @block.sync
        def _(e):
            run("sp", e)


class Rot:
    def __init__(self, items):
        self.items = items
        self.i = 0

    def next(self):
        it = self.items[self.i % len(self.items)]
        self.i += 1
        return it


def build_program(PRE, MAIN, CAP, dbg=False):
    NTOK = PRE + HALO + MAIN
    NSLOT = NEXP * CAP
    ZROW = NSLOT
    NBLKC = MAIN // P
    CB = CAP // P
    NST = MAIN // (ST_TILES * TW)
    HW = HALO + ST_TILES * TW
    nc = bass.Bass("TRN2", target_bir_lowering=False)

    def din(name, shape, dt=F32):
        return nc.dram_tensor(name, list(shape), dt, kind="ExternalInput").ap()

    xT = din("xT", [P, KC, NTOK])
    cosT = din("cosT", [P, NTOK])
    sinT = din("sinT", [P, NTOK])
    vecs_d = din("vecs", [P, NVEC])
    maskT_d = din("maskT", [P, 4, P])
    qdec_d = din("qdec", [P, 4, P])
    kdec_d = din("kdec", [P, 4])
    ident_d = din("ident", [P, P])
    sgmask_d = din("sgmask", [P, P])
    sgwT_d = din("sgwT", [P, 4, P])
    sgb_d = din("sgb", [P, 4, P])
    halom_d = din("halom", [P, 1])
    e_w_in = din("e_w_in", [D, 3072])
    e_w_out = din("e_w_out", [D, D])
    e_ffn_gate = din("e_ffn_gate", [D, D_FF])
    e_ffn_up = din("e_ffn_up", [D, D_FF])
    e_ffn_down = din("e_ffn_down", [D_FF, D])
    o_w_in = din("o_w_in", [D, 2560])
    o_w_out = din("o_w_out", [D, D])
    o_router = din("o_router", [D, NEXP])
    o_exp_gate = din("o_exp_gate", [NEXP, D, D_EXP])
    o_exp_up = din("o_exp_up", [NEXP, D, D_EXP])
    o_exp_down = din("o_exp_down", [NEXP, D_EXP, D])
    fng_d = din("fng", [P, D])
    ut_d = din("ut", [P, P])
    eoff_d = din("eoff", [P, NEXP])
    out_d = nc.dram_tensor("out", [MAIN, D], F32, kind="ExternalOutput").ap()
    dk = dict(kind="ExternalOutput") if dbg else {}
    XG = nc.dram_tensor("xg_scratch", [NSLOT + 72, D], BF16, **dk).ap()
    YS = nc.dram_tensor("ys_scratch", [NSLOT + 40, D], F32, **dk).ap()
    H3D = nc.dram_tensor("h3_scratch", [MAIN, D], F32, **dk).ap()
    if dbg:
        dbg_idx = nc.dram_tensor("dbg_idx", [P, 2, NBLKC], mybir.dt.uint32, kind="ExternalOutput").ap()
        dbg_w = nc.dram_tensor("dbg_w", [P, 2, NBLKC], F32, kind="ExternalOutput").ap()

    cdec = [float((1.0 - 2.0 ** (-5 - h)) ** 128) for h in range(4)]

    with ExitStack() as es:
        pg = Prog(nc, es)

        def sb(name, shape, dt):
            return es.enter_context(nc.sbuf_tensor(name, list(shape), dt))

        ARENA_N = 61696
        ARENA = sb("ARENA", [P, ARENA_N], BF16)
        _ao = [0]
        P1_BUFS = []
        P2_BUFS = []

        def ar(shape, dt):
            n = int(np.prod(shape[1:])) * (2 if dt == F32 else 1)
            assert _ao[0] + n <= ARENA_N, (_ao[0], n)
            a = ARENA[:, _ao[0]:_ao[0] + n]
            _ao[0] += n
            if dt == F32:
                a = a.bitcast(F32)
            if len(shape) == 3:
                a = a.rearrange("p (a b) -> p a b", a=shape[1])
            return a

        def atmp(name, shape, dt, lst=None):
            b = Buf(name)
            (P1_BUFS if lst is None else lst).append(b)
            return ar(shape, dt), b

        HB = ar([P, KC, HW], F32)
        HNB = ar([P, KC, HW], BF16)
        slot_off = [0] + [HALO + i * TW for i in range(ST_TILES)]
        slot_w = [HALO] + [TW] * ST_TILES
        Hbuf = [Buf("H%d" % i) for i in range(1 + ST_TILES)]
        HNbuf = [[Buf("HN%d_%d" % (i, c)) for c in range(KC)] for i in range(1 + ST_TILES)]
        P1_BUFS.extend(Hbuf + [b_ for l_ in HNbuf for b_ in l_])
        Hsem = [pg.new_semc() for _ in range(1 + ST_TILES)]

        def Hs(s, W=None):
            W = slot_w[s] if W is None else W
            return HB[:, :, slot_off[s]:slot_off[s] + W]

        def HNs(s, W=None):
            W = slot_w[s] if W is None else W
            return HNB[:, :, slot_off[s]:slot_off[s] + W]

        WBUF = sb("WBUF", [P, KC * 3072], BF16)
        WIN0 = WBUF[:, :].rearrange("p (c n) -> p c n", c=KC)
        WIN1 = WBUF[:, 0:KC * 2560].rearrange("p (c n) -> p c n", c=KC)
        WOUT = ar([P, KC, D], BF16)
        WINb, WOUTb = Buf("WIN"), Buf("WOUT")
        P1_BUFS.append(WOUTb)
        WINsem, WOUTsem = pg.new_semc(), pg.new_semc()
        G = 2
        FSZ = 3 * KC * G * P
        FS = []
        for i in range(2):
            o0 = i * FSZ
            FS.append(dict(g=WBUF[:, o0:o0 + 2048].rearrange("p (c n) -> p c n", c=KC),
                           u=WBUF[:, o0 + 2048:o0 + 4096].rearrange("p (c n) -> p c n", c=KC),
                           d=WBUF[:, o0 + 4096:o0 + 6144].rearrange("p (j n) -> p j n", j=G),
                           buf=Buf("FS%d" % i), sem=pg.new_semc()))
        fs_rot = Rot(FS)
        _wo = [2 * FSZ]

        def carve(n_bf16, dt=BF16):
            a = WBUF[:, _wo[0]:_wo[0] + n_bf16]
            _wo[0] += n_bf16
            return a if dt == BF16 else a.bitcast(F32)

        VECS = sb("VECS", [P, NVEC], F32)
        MASKT = sb("MASKT", [P, 4, P], F32)
        QDEC = sb("QDEC", [P, 4, P], F32)
        KDEC = sb("KDEC", [P, 4], F32)
        IDB = sb("IDB", [P, P], BF16)
        IDF = sb("IDF", [P, P], F32)
        ONES = sb("ONES", [P, P], F32)
        EPSC = sb("EPSC", [P, 1], F32)
        SGW = sb("SGW", [P, 4, P], BF16)
        SGMASK = sb("SGMASK", [P, P], F32)
        SGB = sb("SGB", [P, 4, P], F32)
        HALOM = sb("HALOM", [P, 1], F32)
        RG = sb("RG", [P, KC, NEXP], F32)
        CONST = Buf("CONST")
        constsem = pg.new_semc()

        def tmp(name, shape, dt):
            return sb(name, shape, dt), Buf(name)

        SQ = [tmp("SQ%d" % i, [P, TW], F32) for i in range(2)]
        sq_rot = Rot(SQ)
        RSTD, RSTDb = tmp("RSTD", [P, TW], F32)
        TA = [tmp("TA%d" % i, [P, TW], F32) for i in range(2)]
        ta_rot = Rot(TA)
        TBt = [tmp("TB%d" % i, [P, TW], F32) for i in range(2)]
        tb_rot = Rot(TBt)
        MEAN, MEANb = tmp("MEAN", [P, TW], F32)
        VAR, VARb = tmp("VAR", [P, TW], F32)
        ROTQ, ROTQb = atmp("ROTQ", [P, 4, TW], BF16)
        _off_rotk = _ao[0]
        ROTK, ROTKb = atmp("ROTK", [P, 4, TW], BF16)
        QD, QDb = atmp("QD", [P, 4, TW], BF16)
        KD, KDb = atmp("KD", [P, 4, TW], BF16)
        MBUF = ARENA[:, _off_rotk:_off_rotk + 4 * (2 + TW) * 2].bitcast(F32).rearrange("p (a b) -> p a b", a=4)
        MBb = [ROTKb, QDb, KDb]
        VT, VTb = atmp("VT", [P, 4, TW], BF16)
        PT = [atmp("PT%d" % i, [P, 4, P], BF16) for i in range(2)]
        pt_rot = Rot(PT)
        SILUG, SILUGb = atmp("SILUG", [P, 4, TW], BF16)
        STATE, STATEb = tmp("STATE", [P, 4, P], F32)
        STATEB, STATEBb = tmp("STATEB", [P, 4, P], BF16)
        CBUF, CBUFb = atmp("CBUF", [P, 4, 30 + TW], BF16)
        DG = [atmp("DG%d" % i, [P, P], BF16) for i in range(4)]
        dg_rot = Rot(DG)
        CARRY0, CARRY0b = tmp("CARRY0", [P, 4, 30], BF16)
        CARRY1, CARRY1b = tmp("CARRY1", [P, 4, 2], F32)
        CONVO = ar([P, 4, TW], F32)
        CONVOb = [Buf("CONVO%d" % i) for i in range(4)]
        P1_BUFS.extend(CONVOb)
        COS = [atmp("COS%d" % i, [P, TW], F32) for i in range(2)]
        SIN = [atmp("SIN%d" % i, [P, TW], F32) for i in range(2)]
        tabsem = [(pg.new_semc(), pg.new_semc()) for _ in range(2)]
        AB = [(carve(G * TW).rearrange("p (j n) -> p j n", j=G), Buf("AB%d" % i)) for i in range(2)]
        ab_rot = Rot(AB)
        SGT = [(carve(TW), Buf("SGT%d" % i)) for i in range(2)]
        sgt_rot = Rot(SGT)
        HNT = [(carve(D), Buf("HNT%d" % i)) for i in range(2)]
        hnt_rot = Rot(HNT)
        H3S = [(carve(2 * D, F32), Buf("H3S%d" % i)) for i in range(2)]
        h3s_rot = Rot(H3S)
        hnt_sem = [pg.new_semc() for _ in range(2)]
        IDXA, IDXAb = tmp("IDXA", [P, NBLKC], mybir.dt.uint32)
        IDXB, IDXBb = tmp("IDXB", [P, NBLKC], mybir.dt.uint32)
        WA, WAb = tmp("WA", [P, NBLKC], F32)
        WB, WBb = tmp("WB", [P, NBLKC], F32)
        RUN, RUNb = tmp("RUN", [P, NEXP], F32)
        UT = sb("UT", [P, P], F32)
        EOFF = sb("EOFF", [P, NEXP], F32)
        XGb, YSb, H3Db = Buf("XG"), Buf("YS"), Buf("H3D")
        xg_sem, ys_sem, h3d_sem = pg.new_semc(), pg.new_semc(), pg.new_semc()
        LG, LGb = tmp("LG", [P, 16], F32)
        RT = {n: tmp("RT_" + n, [P, 8], F32) for n in ("L", "MK1", "L2", "MK2", "G", "POS", "OK", "SL", "T8")}
        RS = {n: tmp("RS_" + n, [P, 1], F32) for n in ("M1", "M2", "D", "E", "W1", "W2", "SA", "SB", "SS")}
        FFN_BUFS = [f["buf"] for f in FS] + [b for _, b in AB] + [b for _, b in SGT] + [b for _, b in HNT] + [b for _, b in H3S]
        FENCE = sb("FENCE", [P, 2], F32)

        def fence(reads, writes):
            pg.op("dve", lambda e: e.memset(FENCE[0:1, 0:1], 0.0), reads, writes)
        E0 = sb("E0", [P, 1], F32)

        PS = [es.enter_context(nc.psum_tensor("ps%d" % i, [P, TW], F32)) for i in range(8)]
        PSb = [Buf("ps%d" % i) for i in range(8)]
        mm_rot = Rot([(PS[i], PSb[i]) for i in range(4)])
        aux_rot = Rot([(PS[i], PSb[i]) for i in (4, 5)])
        acc_rot = Rot([(PS[i], PSb[i]) for i in (6, 7)])
        dn_rot = Rot([(PS[i], PSb[i]) for i in (4, 5, 6, 7)])

        def mm(out, lhsT, rhs, start, stop, reads, writes):
            pg.op("pe", lambda e: e.matmul(out, lhsT, rhs, start=start, stop=stop), reads, writes)

        def tr(out, in_, reads, writes):
            pg.op("pe", lambda e: e.transpose(out, in_, IDB[:]), reads + [CONST], writes)

        def act(out, in_, func, reads, writes, bias=None, scale=None):
            kw = {}
            if bias is not None:
                kw["bias"] = bias
            if scale is not None:
                kw["scale"] = scale
            pg.op("act", lambda e: e.activation(out, in_, func, **kw), reads, writes)

        def tt(out, in0, in1, op, reads, writes):
            pg.op("dve", lambda e: e.tensor_tensor(out, in0, in1, op), reads, writes)

        def ts(out, in0, s1, s2, op0, op1, reads, writes):
            if op1 is None:
                pg.op("dve", lambda e: e.tensor_scalar(out, in0, s1, None, op0), reads, writes)
            else:
                pg.op("dve", lambda e: e.tensor_scalar(out, in0, s1, s2, op0, op1), reads, writes)

        def stt(out, in0, scalar, in1, op0, op1, reads, writes):
            pg.op("dve", lambda e: e.scalar_tensor_tensor(out, in0, scalar, in1, op0, op1), reads, writes)

        def vcopy(out, in_, reads, writes):
            pg.op("dve", lambda e: e.tensor_copy(out, in_), reads, writes)

        def recip(out, in_, reads, writes):
            pg.op("dve", lambda e: e.reciprocal(out, in_), reads, writes)

        def memset(out, val, writes):
            pg.op("dve", lambda e: e.memset(out, val), [], writes)

        def dma(q, out, in_, reads, writes, semc):
            return pg.op(q, lambda e: e.dma_start(out=out, in_=in_), reads, writes, dma_sem=semc)

        def vcol(name, i):
            c = VEC[name] + i
            return VECS[:, c:c + 1]

        for dst, src in ((VECS, vecs_d), (MASKT, maskT_d), (QDEC, qdec_d), (KDEC, kdec_d), (IDF, ident_d),
                         (SGMASK, sgmask_d), (SGB, sgb_d), (HALOM, halom_d)):
            dma("sp", dst[:], src, [], [CONST], constsem)
        dma("sp", RG[:], o_router.rearrange("(c p) e -> p c e", p=P), [], [CONST], constsem)
        dma("sp", UT[:], ut_d, [], [CONST], constsem)
        dma("sp", EOFF[:], eoff_d, [], [CONST], constsem)
        memset(RUN[:], 0.0, [RUNb])
        dma("pool", IDB[:], ident_d, [], [CONST], pg.new_semc())
        memset(ONES[:], 1.0, [CONST])
        memset(EPSC[:], EPS, [CONST])
        memset(E0[:], 0.0, [CONST])
        memset(E0[0:1, :], 1.0, [CONST])
        memset(STATE[:], 0.0, [STATEb])
        memset(STATEB[:], 0.0, [STATEBb])
        memset(CARRY0[:], 0.0, [CARRY0b])
        memset(CARRY1[:], 0.0, [CARRY1b])
        dma("sp", CONVO[:, :, 0:P], sgwT_d, [], CONVOb, constsem)
        tt(SGW[:], CONVO[:, :, 0:P], SGMASK[:].unsqueeze(1).to_broadcast([P, 4, P]), ALU.mult,
           [CONST] + CONVOb, [CONST])
        for c in range(KC):
            ts(RG[:, c, :], RG[:, c, :], vcol("o_norm_ffn", c), None, ALU.mult, None, [CONST], [CONST])

        def load_x(slot, col0, W):
            dma("sp", Hs(slot, W), xT[:, :, col0:col0 + W], [], [Hbuf[slot]], Hsem[slot])

        def load_tabs(par, col0, W):
            dma("sp", COS[par][0][:, :W], cosT[:, col0:col0 + W], [], [COS[par][1]], tabsem[par][0])
            dma("sp", SIN[par][0][:, :W], sinT[:, col0:col0 + W], [], [SIN[par][1]], tabsem[par][1])

        def stats_rstd(srcs, W, scale, reads):
            ps, psb = aux_rot.next()
            n = len(srcs)
            for i, s in enumerate(srcs):
                sq, sqb = sq_rot.next()
                act(sq[:, :W], s, AF.Square, reads, [sqb])
                mm(ps[:, :W], ONES[:], sq[:, :W], i == 0, i == n - 1, [sqb, CONST], [psb])
            act(RSTD[:, :W], ps[:, :W], AF.Sqrt, [psb, CONST], [RSTDb], bias=EPSC[:], scale=scale)
            recip(RSTD[:, :W], RSTD[:, :W], [RSTDb], [RSTDb])

        def rmsnorm(slot, W, gname):
            h = Hs(slot, W)
            stats_rstd([h[:, c, :] for c in range(KC)], W, 1.0 / D, [Hbuf[slot]])
            hn = HNs(slot, W)
            for c in range(KC):
                stt(hn[:, c, :], h[:, c, :], vcol(gname, c), RSTD[:, :W], ALU.mult, ALU.mult,
                    [Hbuf[slot], RSTDb, CONST], [HNbuf[slot][c]])

        def ln_stats(srcs, srcbufs, W, n_feat):
            ps1, ps1b = aux_rot.next()
            ps2, ps2b = aux_rot.next()
            n = len(srcs)
            for i, (s, b) in enumerate(zip(srcs, srcbufs)):
                sq, sqb = sq_rot.next()
                act(sq[:, :W], s, AF.Square, [b], [sqb])
                mm(ps1[:, :W], ONES[:], s, i == 0, i == n - 1, [b, CONST], [ps1b])
                mm(ps2[:, :W], ONES[:], sq[:, :W], i == 0, i == n - 1, [sqb, CONST], [ps2b])
            tb, tbb = tb_rot.next()
            act(MEAN[:, :W], ps1[:, :W], AF.Copy, [ps1b], [MEANb], scale=1.0 / n_feat)
            tt(tb[:, :W], MEAN[:, :W], MEAN[:, :W], ALU.mult, [MEANb], [tbb])
            stt(VAR[:, :W], ps2[:, :W], 1.0 / n_feat, tb[:, :W], ALU.mult, ALU.subtract, [ps2b, tbb], [VARb])
            ts(VAR[:, :W], VAR[:, :W], 0.0, None, ALU.max, None, [VARb], [VARb])
            act(VAR[:, :W], VAR[:, :W], AF.Sqrt, [VARb, CONST], [VARb], bias=EPSC[:], scale=1.0)
            recip(VAR[:, :W], VAR[:, :W], [VARb], [VARb])

        def proj_chunk(wtile, wbuf, col, slot, W):
            ps, psb = mm_rot.next()
            hn = HNs(slot, W)
            for c in range(KC):
                mm(ps[:, :W], wtile[:, c, col:col + P], hn[:, c, :], c == 0, c == KC - 1,
                   [wbuf, HNbuf[slot][c]], [psb])
            return ps, psb

        def rotary(ps, psb, dst, W, par):
            ta, tab = ta_rot.next()
            tb, tbb = tb_rot.next()
            cos, cosb = COS[par]
            sin, sinb = SIN[par]
            tt(ta[:, :W], ps[:, :W], cos[:, :W], ALU.mult, [psb, cosb], [tab])
            tt(tb[0:64, :W], ps[64:128, :W], sin[64:128, :W], ALU.mult, [psb, sinb], [tbb])
            tt(tb[64:128, :W], ps[0:64, :W], sin[0:64, :W], ALU.mult, [psb, sinb], [tbb])
            return ta, tab, tb, tbb

        def Yv(slot):
            o = {0: 1, 1: 2, 2: 1}[slot]
            return HNB[:, :, slot_off[o]:slot_off[o] + TW], HNbuf[o]

        def wout_residual(wtile, wbuf, slot, W):
            Y, Yb = Yv(slot)
            h = Hs(slot, W)
            for oc in range(KC):
                ps, psb = dn_rot.next()
                for c in range(KC):
                    mm(ps[:, :W], wtile[:, c, oc * P:(oc + 1) * P], Y[:, c, :W], c == 0, c == KC - 1,
                       [wbuf, Yb[c]], [psb])
                tt(h[:, oc, :], h[:, oc, :], ps[:, :W], ALU.add, [Hbuf[slot], psb], [Hbuf[slot]])

        def load_w(dst, dstbuf, semc, src, ncols):
            for c in range(KC):
                dma("pool", dst[:, c, :ncols], src[c * P:(c + 1) * P, :], [], [dstbuf], semc)

        BS0 = (ROTK, ROTKb, VT, VTb, KD, KDb)
        BS1 = (ROTQ, ROTQb, QD, QDb, SILUG, SILUGb)

        def mixer0_tile(slot, W, par, mode, stage="12B", bs=None):
            full = mode == "full"
            nblk = W // P
            Y, Yb = Yv(slot)
            WIN = WIN0
            RK, RKb, VTx, VTxb, KDx, KDxb = BS0 if bs is None else bs
            if "1" in stage:
                rmsnorm(slot, W, "e_norm_mix")
            if "2" in stage:
                mixer0_stageA(slot, W, par, mode, full, nblk, WIN, RK, RKb, VTx, VTxb)
            if "B" in stage:
                mixer0_stageB(slot, W, par, mode, full, nblk, WIN, Y, Yb, RK, RKb, VTx, VTxb, KDx, KDxb)

        def mixer0_stageA(slot, W, par, mode, full, nblk, WIN, RK, RKb, VTx, VTxb):
            for h in range(4):
                ps, psb = proj_chunk(WIN, WINb, 512 + h * P, slot, W)
                ta, tab, tb, tbb = rotary(ps, psb, None, W, par)
                tt(RK[:, h, :W], ta[:, :W], tb[:, :W], ALU.add, [tab, tbb], [RKb])
            if full:
                for h in range(4):
                    ps, psb = proj_chunk(WIN, WINb, h * P, slot, W)
                    ta, tab, tb, tbb = rotary(ps, psb, None, W, par)
                    tt(ROTQ[:, h, :W], ta[:, :W], tb[:, :W], ALU.add, [tab, tbb], [ROTQb])
                for h in range(4):
                    tt(QD[:, h, :W].rearrange("p (b c) -> p b c", c=P),
                       ROTQ[:, h, :W].rearrange("p (b c) -> p b c", c=P),
                       QDEC[:, h, :].unsqueeze(1).to_broadcast([P, nblk, P]), ALU.mult,
                       [ROTQb, CONST], [QDb])
            hn = HNs(slot, W)
            for b in range(nblk):
                ps, psb = mm_rot.next()
                for c in range(KC):
                    mm(ps[:, :], hn[:, c, b * P:(b + 1) * P], WIN[:, c, 1024:1536], c == 0, c == KC - 1,
                       [WINb, HNbuf[slot][c]], [psb])
                act(VTx[:, b, :], ps[:, :], AF.Copy, [psb], [VTxb])
            if full:
                for h in range(4):
                    ps, psb = proj_chunk(WIN, WINb, 1536 + h * P, slot, W)
                    act(SILUG[:, h, :W], ps[:, :W], AF.Silu, [psb], [SILUGb])
            if mode in ("full", "kvab"):
                vcopy(CBUF[:, :, 0:30], CARRY0[:], [CARRY0b], [CBUFb])
                for c in range(4):
                    psa, psab = proj_chunk(WIN, WINb, 2048 + c * P, slot, W)
                    psg, psgb = proj_chunk(WIN, WINb, 2560 + c * P, slot, W)
                    ta, tab = ta_rot.next()
                    act(ta[:, :W], psg[:, :W], AF.Sigmoid, [psgb], [tab])
                    tt(CBUF[:, c, 30:30 + W], psa[:, :W], ta[:, :W], ALU.mult, [psab, tab], [CBUFb])
                vcopy(CARRY0[:], CBUF[:, :, W:W + 30], [CBUFb], [CARRY0b])
        def mixer0_stageB(slot, W, par, mode, full, nblk, WIN, Y, Yb, RK, RKb, VTx, VTxb, KDx, KDxb):
            for b in range(nblk):
                ps, psb = aux_rot.next()
                psv = ps[:].bitcast(BF16)
                for h in range(4):
                    tr(psv[:, h * P:(h + 1) * P], RK[:, h, b * P:(b + 1) * P], [RKb], [psb])
                tt(KDx[:, b, :].rearrange("p (h d) -> p h d", d=P),
                   psv[:, 0:512].rearrange("p (h d) -> p h d", d=P),
                   KDEC[:].unsqueeze(2).to_broadcast([P, 4, P]), ALU.mult, [psb, CONST], [KDxb])
            for b in range(nblk):
                cols = slice(b * P, (b + 1) * P)
                if full:
                    ps, psb = aux_rot.next()
                    for h in range(4):
                        mm(ps[:, h * P:(h + 1) * P], RK[:, h, cols], ROTQ[:, h, cols], True, True,
                           [RKb, ROTQb], [psb])
                    pt, ptb = pt_rot.next()
                    tt(pt[:], ps[:].rearrange("p (h c) -> p h c", c=P), MASKT[:], ALU.mult, [psb, CONST], [ptb])
                    po, pob = acc_rot.next()
                    for h in range(4):
                        mm(po[:, h * P:(h + 1) * P], VTx[:, b, h * P:(h + 1) * P], pt[:, h, :], True, False,
                           [VTxb, ptb], [pob])
                        mm(po[:, h * P:(h + 1) * P], STATEB[:, h, :], QD[:, h, cols], False, True,
                           [STATEBb, QDb], [pob])
                    act(CONVO[:, :, cols], po[:].rearrange("p (h c) -> p h c", c=P), AF.Copy, [pob], CONVOb)
                pk, pkb = acc_rot.next()
                for h in range(4):
                    mm(pk[:, h * P:(h + 1) * P], KDx[:, b, h * P:(h + 1) * P], VTx[:, b, h * P:(h + 1) * P],
                       True, True, [KDxb, VTxb], [pkb])
                for h in range(4):
                    stt(STATE[:, h, :], STATE[:, h, :], cdec[h], pk[:, h * P:(h + 1) * P], ALU.mult, ALU.add,
                        [STATEb, pkb], [STATEb])
                act(STATEB[:], STATE[:], AF.Copy, [STATEb], [STATEBb])
            if not full:
                return
            for h in range(4):
                r = CONVO[:, h, :W]
                ln_stats([r], [CONVOb[h]], W, P)
                ta, tab = ta_rot.next()
                tt(ta[:, :W], r, MEAN[:, :W], ALU.subtract, [CONVOb[h], MEANb], [tab])
                tt(ta[:, :W], ta[:, :W], VAR[:, :W], ALU.mult, [tab, VARb], [tab])
                stt(Y[:, h, :W], ta[:, :W], vcol("e_ret_gn", h), SILUG[:, h, :W], ALU.mult, ALU.mult,
                    [tab, SILUGb, CONST], [Yb[h]])
            for c in range(4):
                ps, psb = mm_rot.next()
                for j in range(CONV_K):
                    dg, dgb = dg_rot.next()
                    wc = VEC["e_dw_w"] + j * 4 + c
                    ts(dg[:, :], IDB[:], VECS[:, wc:wc + 1], None, ALU.mult, None, [CONST], [dgb])
                    mm(ps[:, :W], dg[:, :], CBUF[:, c, j:j + W], j == 0, j == CONV_K - 1, [dgb, CBUFb], [psb])
                ts(CONVO[:, c, :W], ps[:, :W], vcol("e_dw_b", c), None, ALU.add, None, [psb, CONST], [CONVOb[c]])
            ln_stats([CONVO[:, c, :W] for c in range(4)], CONVOb, W, 512)
            for c in range(4):
                ta, tab = ta_rot.next()
                tt(ta[:, :W], CONVO[:, c, :W], MEAN[:, :W], ALU.subtract, [CONVOb[c], MEANb], [tab])
                tt(ta[:, :W], ta[:, :W], VAR[:, :W], ALU.mult, [tab, VARb], [tab])
                act(Y[:, 4 + c, :W], ta[:, :W], AF.Silu, [tab, CONST], [Yb[4 + c]],
                    bias=vcol("e_cv_ln_b", c), scale=vcol("e_cv_ln_g", c))
            wout_residual(WOUT, WOUTb, slot, W)

        def gelu_from_psum(ps, psb, W, out, outreads, outwrites):
            sq, sqb = sq_rot.next()
            ta, tab = ta_rot.next()
            act(sq[:, :W], ps[:, :W], AF.Square, [psb], [sqb])
            ts(ta[:, :W], sq[:, :W], 0.044715, 1.0, ALU.mult, ALU.add, [sqb], [tab])
            tt(ta[:, :W], ta[:, :W], ps[:, :W], ALU.mult, [tab, psb], [tab])
            act(ta[:, :W], ta[:, :W], AF.Sigmoid, [tab], [tab], scale=GELU_C)
            tt(out, ps[:, :W], ta[:, :W], ALU.mult, [psb, tab] + outreads, outwrites)

        def mixer1_tile(slot, W, mode):
            full = mode == "full"
            nblk = W // P
            Y, Yb = Yv(slot)
            WIN = WIN1
            rmsnorm(slot, W, "o_norm_mix")
            if full:
                for c in range(4):
                    ps, psb = proj_chunk(WIN, WINb, c * P, slot, W)
                    gelu_from_psum(ps, psb, W, SILUG[:, c, :W], [], [SILUGb])
                for c in range(4):
                    ps, psb = proj_chunk(WIN, WINb, 512 + c * P, slot, W)
                    gelu_from_psum(ps, psb, W, CONVO[:, c, :W], [], [CONVOb[c]])
                ln_stats([CONVO[:, c, :W] for c in range(4)], CONVOb, W, 512)
                for c in range(4):
                    ta, tab = ta_rot.next()
                    tt(ta[:, :W], CONVO[:, c, :W], MEAN[:, :W], ALU.subtract, [CONVOb[c], MEANb], [tab])
                    tt(ta[:, :W], ta[:, :W], VAR[:, :W], ALU.mult, [tab, VARb], [tab])
                    act(ROTQ[:, c, :W], ta[:, :W], AF.Identity, [tab, CONST], [ROTQb],
                        bias=vcol("o_sg_ln_b", c), scale=vcol("o_sg_ln_g", c))
                for b in range(nblk):
                    ps, psb = aux_rot.next()
                    psv = ps[:].bitcast(BF16)
                    for g in range(4):
                        tr(psv[:, g * P:(g + 1) * P], ROTQ[:, g, b * P:(b + 1) * P], [ROTQb], [psb])
                    vcopy(VT[:, b, :], psv[:, 0:512], [psb], [VTb])
                for b in range(nblk):
                    cols = slice(b * P, (b + 1) * P)
                    ps, psb = acc_rot.next()
                    for g in range(4):
                        mm(ps[:, g * P:(g + 1) * P], VT[:, b, g * P:(g + 1) * P], SGW[:, g, :], True, True,
                           [VTb, CONST], [psb])
                    ta, tab = ta_rot.next()
                    tav = ta[:].rearrange("p (g c) -> p g c", c=P)
                    tt(tav, ps[:].rearrange("p (g c) -> p g c", c=P), SGB[:], ALU.add, [psb, CONST], [tab])
                    tt(Y[:, 0:4, cols], tav, SILUG[:, :, cols], ALU.mult, [tab, SILUGb], Yb[0:4])
            vcopy(MBUF[:, :, 0:2], CARRY1[:], [CARRY1b], MBb)
            for c in range(4):
                psc, pscb = proj_chunk(WIN, WINb, 1536 + c * P, slot, W)
                psh, pshb = proj_chunk(WIN, WINb, 2048 + c * P, slot, W)
                ta, tab = ta_rot.next()
                act(ta[:, :W], psc[:, :W], AF.Copy, [pscb], [tab])
                tt(MBUF[:, c, 2:2 + W], psh[:, :W], ta[:, :W], ALU.mult, [pshb, tab], MBb)
                if full:
                    psg, psgb = proj_chunk(WIN, WINb, 1024 + c * P, slot, W)
                    tb, tbb = tb_rot.next()
                    w0 = VEC["o_sc_w"] + c
                    ts(tb[:, :W], MBUF[:, c, 0:W], VECS[:, w0:w0 + 1], None, ALU.mult, None, MBb + [CONST], [tbb])
                    stt(tb[:, :W], MBUF[:, c, 1:1 + W], VECS[:, w0 + 4:w0 + 5], tb[:, :W], ALU.mult, ALU.add,
                        MBb + [tbb, CONST], [tbb])
                    stt(tb[:, :W], MBUF[:, c, 2:2 + W], VECS[:, w0 + 8:w0 + 9], tb[:, :W], ALU.mult, ALU.add,
                        MBb + [tbb, CONST], [tbb])
                    tt(Y[:, 4 + c, :W], tb[:, :W], psg[:, :W], ALU.mult, [tbb, psgb], [Yb[4 + c]])
            if full:
                vcopy(CARRY1[:], MBUF[:, :, W:W + 2], MBb, [CARRY1b])
                wout_residual(WOUT, WOUTb, slot, W)
            else:
                ts(CARRY1[:], MBUF[:, :, W:W + 2], HALOM[:, 0:1], None, ALU.mult, None, MBb + [CONST], [CARRY1b])

        def load_group(fs, wg, wu, wd, f0):
            for c in range(KC):
                dma("pool", fs["g"][:, c, :], wg[c * P:(c + 1) * P, f0:f0 + G * P], [], [fs["buf"]], fs["sem"])
            for c in range(KC):
                dma("pool", fs["u"][:, c, :], wu[c * P:(c + 1) * P, f0:f0 + G * P], [], [fs["buf"]], fs["sem"])
            for j in range(G):
                dma("pool", fs["d"][:, j, :], wd[f0 + j * P:f0 + (j + 1) * P, :], [], [fs["buf"]], fs["sem"])

        def ffn_group_compute(fs, slot, W, gw=None):
            hn = HNs(slot, W)
            h = Hs(slot, W)
            ab, abb = ab_rot.next()
            for j in range(G):
                pg_, pgb = mm_rot.next()
                pu_, pub = mm_rot.next()
                for c in range(KC):
                    mm(pg_[:, :W], fs["g"][:, c, j * P:(j + 1) * P], hn[:, c, :], c == 0, c == KC - 1,
                       [fs["buf"], HNbuf[slot][c]], [pgb])
                for c in range(KC):
                    mm(pu_[:, :W], fs["u"][:, c, j * P:(j + 1) * P], hn[:, c, :], c == 0, c == KC - 1,
                       [fs["buf"], HNbuf[slot][c]], [pub])
                if gw is None:
                    sg, sgb_ = sgt_rot.next()
                    act(sg[:, :W], pg_[:, :W], AF.Silu, [pgb], [sgb_])
                    tt(ab[:, j, :W], pu_[:, :W], sg[:, :W], ALU.mult, [pub, sgb_], [abb])
                else:
                    gwt, gwb, gcol = gw
                    ta, tab = ta_rot.next()
                    act(ta[:, :W], pg_[:, :W], AF.Silu, [pgb], [tab])
                    tt(ta[:, :W], pu_[:, :W], ta[:, :W], ALU.mult, [pub, tab], [tab])
                    tt(ab[:, j, :W], ta[:, :W], gwt[:, gcol:gcol + W], ALU.mult, [tab, gwb], [abb])
            for oc in range(KC):
                ps, psb = dn_rot.next()
                for j in range(G):
                    mm(ps[:, :W], fs["d"][:, j, oc * P:(oc + 1) * P], ab[:, j, :W], j == 0, j == G - 1,
                       [fs["buf"], abb], [psb])
                tt(h[:, oc, :], h[:, oc, :], ps[:, :W], ALU.add, [Hbuf[slot], psb], [Hbuf[slot]])

        _breg = {}

        def bound_reg(e):
            if "r" not in _breg:
                _breg["r"] = e.to_reg(NSLOT - 1)
            return _breg["r"]

        def router_tile(slot, W, blk0):
            h = Hs(slot, W)
            for b in range(W // P):
                cols = slice(b * P, (b + 1) * P)
                ps, psb = aux_rot.next()
                for c in range(KC):
                    mm(ps[:, 0:NEXP], h[:, c, cols], RG[:, c, :], c == 0, c == KC - 1, [Hbuf[slot], CONST], [psb])
                mm(ps[:, 8:9], RSTD[:, cols], E0[:], True, True, [RSTDb, CONST], [psb])
                act(LG[:, 0:9], ps[:, 0:9], AF.Copy, [psb], [LGb])
                L, Lb = RT["L"]
                MK1, MK1b = RT["MK1"]
                L2, L2b = RT["L2"]
                MK2, MK2b = RT["MK2"]
                M1, M1b = RS["M1"]
                M2, M2b = RS["M2"]
                Dd, Db = RS["D"]
                Ee, Eb = RS["E"]
                W1, W1b = RS["W1"]
                W2, W2b = RS["W2"]
                ts(L[:], LG[:, 0:8], LG[:, 8:9], None, ALU.mult, None, [LGb], [Lb])
                pg.op("dve", lambda e: e.tensor_reduce(M1[:], L[:], mybir.AxisListType.X, ALU.max), [Lb], [M1b])
                ts(MK1[:], L[:], M1[:, 0:1], None, ALU.is_equal, None, [Lb, M1b], [MK1b])
                stt(L2[:], MK1[:], -1e30, L[:], ALU.mult, ALU.add, [MK1b, Lb], [L2b])
                pg.op("dve", lambda e: e.tensor_reduce(M2[:], L2[:], mybir.AxisListType.X, ALU.max), [L2b], [M2b])
                ts(MK2[:], L2[:], M2[:, 0:1], None, ALU.is_equal, None, [L2b, M2b], [MK2b])
                tt(Dd[:], M2[:], M1[:], ALU.subtract, [M1b, M2b], [Db])
                act(Ee[:], Dd[:], AF.Exp, [Db], [Eb])
                ts(W1[:], Ee[:], 1.0, None, ALU.add, None, [Eb], [W1b])
                recip(W1[:], W1[:], [W1b], [W1b])
                tt(W2[:], Ee[:], W1[:], ALU.mult, [Eb, W1b], [W2b])
                blkg = blk0 + b
                MS, MSb = RT["G"]
                POS, POSb = RT["POS"]
                OK8, OK8b = RT["OK"]
                SL, SLb = RT["SL"]
                T8, T8b = RT["T8"]
                SA, SAb = RS["SA"]
                SBs, SBsb = RS["SB"]
                tt(MS[:], MK1[:], MK2[:], ALU.add, [MK1b, MK2b], [MSb])
                ps2, ps2b = aux_rot.next()
                mm(ps2[:, 0:NEXP], UT[:], MS[:], True, True, [CONST, MSb], [ps2b])
                mm(ps2[:, 8:16], ONES[:], MS[:], True, True, [CONST, MSb], [ps2b])
                tt(POS[:], ps2[:, 0:NEXP], RUN[:], ALU.add, [ps2b, RUNb], [POSb])
                tt(RUN[:], RUN[:], ps2[:, 8:16], ALU.add, [ps2b, RUNb], [RUNb])
                ts(OK8[:], POS[:], float(CAP), None, ALU.is_lt, None, [POSb], [OK8b])
                tt(SL[:], POS[:], EOFF[:], ALU.add, [POSb, CONST], [SLb])
                stt(SL[:], SL[:], float(-ZROW), OK8[:], ALU.add, ALU.mult, [SLb, OK8b], [SLb])
                ts(SL[:], SL[:], float(ZROW), None, ALU.add, None, [SLb], [SLb])
                tt(T8[:], MK1[:], SL[:], ALU.mult, [MK1b, SLb], [T8b])
                pg.op("dve", lambda e: e.tensor_reduce(SA[:], T8[:], mybir.AxisListType.X, ALU.add), [T8b], [SAb])
                vcopy(IDXA[:, blkg:blkg + 1], SA[:], [SAb], [IDXAb])
                tt(T8[:], MK2[:], SL[:], ALU.mult, [MK2b, SLb], [T8b])
                pg.op("dve", lambda e: e.tensor_reduce(SBs[:], T8[:], mybir.AxisListType.X, ALU.add), [T8b], [SBsb])
                vcopy(IDXB[:, blkg:blkg + 1], SBs[:], [SBsb], [IDXBb])
                vcopy(WA[:, blkg:blkg + 1], W1[:], [W1b], [WAb])
                vcopy(WB[:, blkg:blkg + 1], W2[:], [W2b], [WBb])
                hn = HNs(slot, W)
                pt_, ptb_ = aux_rot.next()
                ptv = pt_[:].bitcast(BF16)
                for c in range(KC):
                    tr(ptv[:, c * P:(c + 1) * P], hn[:, c, cols], [HNbuf[slot][c]], [ptb_])
                hnt, hntb = hnt_rot.next()
                act(hnt[:, :], ptv[:, 0:D], AF.Copy, [ptb_], [hntb])
                for idx, idxb in ((IDXA, IDXAb), (IDXB, IDXBb)):
                    iap = idx[:, blkg:blkg + 1]
                    pg.op("pool", (lambda iap, hnt: lambda e: e.indirect_dma_start(
                        out=XG, out_offset=bass.IndirectOffsetOnAxis(iap, 0), in_=hnt[:, :], in_offset=None,
                        bounds_check=bound_reg(e), oob_is_err=False))(iap, hnt), [hntb, idxb], [XGb], dma_sem=xg_sem)
                pa, pab = mm_rot.next()
                pb, pbb = mm_rot.next()
                for c in range(KC):
                    pp, ppb = (pa, pab) if c < 4 else (pb, pbb)
                    mm(pp[:, (c % 4) * P:(c % 4 + 1) * P], h[:, c, cols], IDF[:], True, True, [Hbuf[slot], CONST], [ppb])
                h3s, h3sb = h3s_rot.next()
                act(h3s[:, 0:512], pa[:, :], AF.Copy, [pab], [h3sb])
                vcopy(h3s[:, 512:1024], pb[:, :], [pbb], [h3sb])
                dma("sp", H3D[blkg * P:(blkg + 1) * P, :], h3s[:, :], [h3sb], [H3Db], h3d_sem)

        n_pre = PRE // TW
        out_sem = pg.new_semc()
        out_ops = []
        tab_i = [0]

        def next_par():
            p = tab_i[0] % 2
            tab_i[0] += 1
            return p

        for st in range(NST):
            main_slots = list(range(1, 1 + ST_TILES))
            main_col0 = [PRE + HALO + (st * ST_TILES + i) * TW for i in range(ST_TILES)]
            fence([], [WINb] + FFN_BUFS)
            load_w(WIN0, WINb, WINsem, e_w_in, 3072)
            load_w(WOUT, WOUTb, WOUTsem, e_w_out, D)
            seq = []
            if st == 0:
                for i in range(n_pre):
                    seq.append((1 + (i % ST_TILES), i * TW, TW, "kvab" if i == n_pre - 1 else "kv"))
                seq.append((0, PRE, HALO, "full"))
            for i in range(ST_TILES):
                seq.append((main_slots[i], main_col0[i], TW, "full"))
            pars = [next_par() for _ in seq]
            done_x, done_t = set(), set()

            def ensure_x(i):
                if i < len(seq) and i not in done_x:
                    done_x.add(i)
                    load_x(seq[i][0], seq[i][1], seq[i][2])

            def ensure_t(i):
                if i < len(seq) and i not in done_t:
                    done_t.add(i)
                    load_tabs(pars[i], seq[i][1], seq[i][2])

            npf = sum(1 for t in seq if t[3] != "full")

            def targs(i):
                s_, c0_, W_, m_ = seq[i]
                return (s_, W_, pars[i], m_)

            for i in range(min(2, len(seq))):
                ensure_x(i)
                ensure_t(i)
            if npf > 0:
                mixer0_tile(*targs(0), stage="1", bs=BS0)
                ensure_x(2)
                for k in range(npf):
                    bs = BS0 if k % 2 == 0 else BS1
                    if k + 1 < npf:
                        mixer0_tile(*targs(k + 1), stage="1", bs=(BS0 if (k + 1) % 2 == 0 else BS1))
                        ensure_x(k + 3)
                    mixer0_tile(*targs(k), stage="2", bs=bs)
                    ensure_t(k + 2)
                    if k >= 1:
                        mixer0_tile(*targs(k - 1), stage="B", bs=(BS0 if (k - 1) % 2 == 0 else BS1))
                mixer0_tile(*targs(npf - 1), stage="B", bs=(BS0 if (npf - 1) % 2 == 0 else BS1))
            for k in range(npf, len(seq)):
                ensure_x(k)
                ensure_t(k)
                mixer0_tile(*targs(k))
                ensure_x(k + 1)
                ensure_t(k + 1)
                ensure_x(k + 2)
                ensure_t(k + 2)
            tiles = ([(0, HALO)] if st == 0 else []) + [(s, TW) for s in main_slots]
            ngrp = D_FF // (G * P)
            fence([], [WINb] + FFN_BUFS)
            fsl = [fs_rot.next() for _ in range(ngrp)]
            load_group(fsl[0], e_ffn_gate, e_ffn_up, e_ffn_down, 0)
            for (s, W) in tiles:
                rmsnorm(s, W, "e_norm_ffn")
            for gi in range(ngrp):
                if gi + 1 < ngrp:
                    load_group(fsl[gi + 1], e_ffn_gate, e_ffn_up, e_ffn_down, (gi + 1) * G * P)
                for (s, W) in tiles:
                    ffn_group_compute(fsl[gi], s, W)
            fence([], [WINb] + FFN_BUFS)
            load_w(WIN1, WINb, WINsem, o_w_in, 2560)
            load_w(WOUT, WOUTb, WOUTsem, o_w_out, D)
            for (s, W) in tiles:
                mixer1_tile(s, W, "carry" if s == 0 else "full")
            fence([], [WINb] + FFN_BUFS)
            for ti, (s, W) in enumerate([(s_, TW) for s_ in main_slots]):
                rmsnorm(s, W, "o_norm_ffn")
                router_tile(s, W, (st * ST_TILES + ti) * (TW // P))

        _ao[0] = 0
        FNG, FNGb = atmp("FNG", [P, D], F32, P2_BUFS)
        _p2base = _ao[0]
        YTM = ar([P, CB, D], F32)
        YTMb = [Buf("YTM%d" % i) for i in range(CB)]
        XT = ar([P, KC, CAP], BF16)
        XTb = Buf("XT")
        XGT = [atmp("XGT%d" % i, [P, D], BF16, P2_BUFS) for i in range(2)]
        xgt_sem = [pg.new_semc() for _ in range(2)]
        NFIN = 8
        fin_sem = [pg.new_semc() for _ in range(NFIN)]
        ya_sem = [pg.new_semc() for _ in range(NFIN)]
        yb_sem = [pg.new_semc() for _ in range(NFIN)]
        fng_sem = pg.new_semc()
        P2_BUFS.extend(YTMb + [XTb])
        fence([], P1_BUFS + P2_BUFS + [WINb] + FFN_BUFS)
        dma("sp", FNG[:, :], fng_d, [], [FNGb], fng_sem)
        zt, ztb = ta_rot.next()
        memset(zt[0:1, :], 0.0, [ztb])
        dma("sp", YS[ZROW:ZROW + 1, 0:TW], zt[0:1, :], [ztb], [YSb], ys_sem)
        dma("sp", YS[ZROW:ZROW + 1, TW:D], zt[0:1, :], [ztb], [YSb], ys_sem)

        slot_tiles = []
        n0 = 0
        while n0 < CAP:
            w = min(TW, CAP - n0)
            slot_tiles.append((n0, w))
            n0 += w
        ngrp = D_EXP // (G * P)
        allg = [(e, gi) for e in range(NEXP) for gi in range(ngrp)]
        fsl = [fs_rot.next() for _ in allg]
        load_group(fsl[0], o_exp_gate[0], o_exp_up[0], o_exp_down[0], 0)
        xi = 0
        for k, (e, gi) in enumerate(allg):
            if k + 1 < len(allg):
                e2, g2 = allg[k + 1]
                load_group(fsl[k + 1], o_exp_gate[e2], o_exp_up[e2], o_exp_down[e2], g2 * G * P)
            fs = fsl[k]
            if gi == 0:
                for b in range(CB):
                    par = xi % 2
                    xi += 1
                    xgt, xgtb = XGT[par]
                    r0 = e * CAP + b * P
                    dma("sp", xgt[:, :], XG[r0:r0 + P, :], [XGb], [xgtb], xgt_sem[par])
                    pt_, ptb_ = aux_rot.next()
                    ptv = pt_[:].bitcast(BF16)
                    for c in range(KC):
                        tr(ptv[:, c * P:(c + 1) * P], xgt[:, c * P:(c + 1) * P], [xgtb], [ptb_])
                    act(XT[:, :, b * P:(b + 1) * P], ptv[:, 0:D].rearrange("p (c s) -> p c s", c=KC), AF.Copy,
                        [ptb_], [XTb])
            for (n0, W) in slot_tiles:
                ab, abb = ab_rot.next()
                for j in range(G):
                    pg_, pgb = mm_rot.next()
                    pu_, pub = mm_rot.next()
                    for c in range(KC):
                        mm(pg_[:, :W], fs["g"][:, c, j * P:(j + 1) * P], XT[:, c, n0:n0 + W], c == 0, c == KC - 1,
                           [fs["buf"], XTb], [pgb])
                    for c in range(KC):
                        mm(pu_[:, :W], fs["u"][:, c, j * P:(j + 1) * P], XT[:, c, n0:n0 + W], c == 0, c == KC - 1,
                           [fs["buf"], XTb], [pub])
                    sg, sgb_ = sgt_rot.next()
                    act(sg[:, :W], pg_[:, :W], AF.Silu, [pgb], [sgb_])
                    tt(ab[:, j, :W], pu_[:, :W], sg[:, :W], ALU.mult, [pub, sgb_], [abb])
                for sbk in range(W // P):
                    blk = n0 // P + sbk
                    for dh in range(2):
                        ps, psb = dn_rot.next()
                        for j in range(G):
                            mm(ps[:, :], ab[:, j, sbk * P:(sbk + 1) * P], fs["d"][:, j, dh * TW:(dh + 1) * TW],
                               j == 0, j == G - 1, [fs["buf"], abb], [psb])
                        ydst = YTM[:, blk, dh * TW:(dh + 1) * TW]
                        if gi == 0:
                            vcopy(ydst, ps[:, :], [psb], [YTMb[blk]])
                        else:
                            tt(ydst, ydst, ps[:, :], ALU.add, [YTMb[blk], psb], [YTMb[blk]])
            if gi == ngrp - 1:
                for b in range(CB):
                    r0 = e * CAP + b * P
                    dma("sp", YS[r0:r0 + P, :], YTM[:, b, :], [YTMb[b]], [YSb], ys_sem)

        SS, SSb = RS["SS"]
        assert _p2base + 3 * NFIN * 2 * D <= ARENA_N
        fviews = [ARENA[:, _p2base + i * 2 * D:_p2base + (i + 1) * 2 * D].bitcast(F32) for i in range(3 * NFIN)]
        H3T = [(fviews[3 * i], Buf("H3T%d" % i)) for i in range(NFIN)]
        YA = [(fviews[3 * i + 1], Buf("YA%d" % i)) for i in range(NFIN)]
        YB = [(fviews[3 * i + 2], Buf("YB%d" % i)) for i in range(NFIN)]
        fence([], YTMb + [XTb] + [b_ for _, b_ in XGT] + [b_ for _, b_ in H3T + YA + YB])
        for j in range(NBLKC):
            par = j % NFIN
            h3t, h3tb = H3T[par]
            ya, yab = YA[par]
            yb, ybb = YB[par]
            dma("sp", h3t[:, :], H3D[j * P:(j + 1) * P, :], [H3Db], [h3tb], fin_sem[par])
            for (yt, ytb, idx, idxb, ysm) in ((ya, yab, IDXA, IDXAb, ya_sem[par]), (yb, ybb, IDXB, IDXBb, yb_sem[par])):
                iap = idx[:, j:j + 1]
                pg.op("pool", (lambda iap, yt: lambda e: e.indirect_dma_start(
                    out=yt[:, :], out_offset=None, in_=YS, in_offset=bass.IndirectOffsetOnAxis(iap, 0)))(iap, yt),
                    [YSb, idxb], [ytb], dma_sem=ysm)
            stt(h3t[:, :], ya[:, :], WA[:, j:j + 1], h3t[:, :], ALU.mult, ALU.add, [yab, WAb, h3tb], [h3tb])
            stt(h3t[:, :], yb[:, :], WB[:, j:j + 1], h3t[:, :], ALU.mult, ALU.add, [ybb, WBb, h3tb], [h3tb])
            memset(SS[:], 0.0, [SSb])
            pg.op("act", (lambda ya, h3t: lambda e: e.activation(ya[:, :], h3t[:, :], AF.Square, accum_out=SS[:, 0:1]))(ya, h3t),
                  [h3tb, SSb], [yab, SSb])
            act(SS[:], SS[:], AF.Sqrt, [SSb, CONST], [SSb], bias=EPSC[:], scale=1.0 / D)
            recip(SS[:], SS[:], [SSb], [SSb])
            stt(h3t[:, :], h3t[:, :], SS[:, 0:1], FNG[:, :], ALU.mult, ALU.mult, [h3tb, SSb, FNGb], [h3tb])
            out_ops.append(dma("sp", out_d[j * P:(j + 1) * P, :], h3t[:, :], [h3tb], [], out_sem))
        if dbg:
            out_ops.append(dma("sp", dbg_idx[:, 0, :], IDXA[:], [IDXAb], [], out_sem))
            out_ops.append(dma("sp", dbg_idx[:, 1, :], IDXB[:], [IDXBb], [], out_sem))
            out_ops.append(dma("sp", dbg_w[:, 0, :], WA[:], [WAb], [], out_sem))
            out_ops.append(dma("sp", dbg_w[:, 1, :], WB[:], [WBb], [], out_sem))
        pg.op("sp", lambda e: e.wait_ge(out_sem.sem, out_sem.count), [], [])
        pg.emit()
    return nc


def _vec_cols(v):
    return np.ascontiguousarray(v.reshape(-1, P).T)


def host_inputs(inputs, PRE, MAIN, CAP, n_seq_cores, batch):
    x = np.asarray(inputs["x"], np.float32)
    B, S, _ = x.shape
    NTOK = PRE + HALO + MAIN
    g = lambda k: np.asarray(inputs[k], np.float32)
    vecs = np.zeros((P, NVEC), np.float32)

    def put(name, arr):
        a = _vec_cols(arr)
        vecs[:, VEC[name]:VEC[name] + a.shape[1]] = a

    put("e_norm_mix", g("e_norm_mix")[0]); put("e_ret_gn", g("e_ret_gn")[0]); put("e_dw_b", g("e_dw_b")[0])
    put("e_cv_ln_g", g("e_cv_ln_g")[0]); put("e_cv_ln_b", g("e_cv_ln_b")[0]); put("e_norm_ffn", g("e_norm_ffn")[0])
    put("o_norm_mix", g("o_norm_mix")[0]); put("o_sg_ln_g", g("o_sg_ln_g")[0]); put("o_sg_ln_b", g("o_sg_ln_b")[0])
    put("o_norm_ffn", g("o_norm_ffn")[0]); put("final_norm", g("final_norm"))
    dw = g("e_dw_w")[0]
    vecs[:, VEC["e_dw_w"]:VEC["e_dw_w"] + 124] = dw.reshape(31, 4, P).transpose(2, 0, 1).reshape(P, 124)
    sc = g("o_sc_w")[0]
    vecs[:, VEC["o_sc_w"]:VEC["o_sc_w"] + 12] = sc.reshape(3, 4, P).transpose(2, 0, 1).reshape(P, 12)

    scale = np.float32(128.0 ** -0.5)
    idx = np.arange(P)
    maskT = np.zeros((P, 4, P), np.float32)
    qdec = np.zeros((P, 4, P), np.float32)
    kdec = np.zeros((P, 4), np.float32)
    for h in range(4):
        gam = 1.0 - 2.0 ** (-5 - h)
        l = idx[:, None]; c = idx[None, :]
        m = (gam ** np.abs(c - l)) * ((l // 64) <= (c // 64)) * scale
        maskT[:, h, :] = m
        qdec[:, h, :] = (scale * gam ** (idx + 1.0))[None, :]
        kdec[:, h] = gam ** (127.0 - idx)
    ident = np.eye(P, dtype=np.float32)
    pch = idx // 64
    sgmask = (pch[None, :] >= pch[:, None]).astype(np.float32)
    sgwT = np.ascontiguousarray(g("o_sg_w")[0].transpose(2, 0, 1))
    sgb = np.ascontiguousarray(np.broadcast_to(g("o_sg_b")[0][None], (P, 4, P)))
    half = 64
    freqs = (10000.0 ** (-np.arange(half, dtype=np.float32) / half)).astype(np.float32)

    fng = np.ascontiguousarray(np.broadcast_to(g("final_norm")[None, :], (P, D)))
    ut = (idx[:, None] < idx[None, :]).astype(np.float32)
    eoff = np.ascontiguousarray(np.broadcast_to((np.arange(NEXP, dtype=np.float32) * CAP)[None, :], (P, NEXP)))
    shared = dict(fng=fng, ut=ut, eoff=eoff, vecs=vecs, maskT=maskT, qdec=qdec, kdec=kdec, ident=ident, sgmask=sgmask, sgwT=sgwT, sgb=sgb,
                  e_w_in=g("e_w_in")[0], e_w_out=g("e_w_out")[0], e_ffn_gate=g("e_ffn_gate")[0],
                  e_ffn_up=g("e_ffn_up")[0], e_ffn_down=g("e_ffn_down")[0], o_w_in=g("o_w_in")[0],
                  o_w_out=g("o_w_out")[0], o_router=g("o_router")[0], o_exp_gate=g("o_exp_gate")[0],
                  o_exp_up=g("o_exp_up")[0], o_exp_down=g("o_exp_down")[0])
    maps = []
    for b in range(batch):
        for q in range(n_seq_cores):
            s0 = q * MAIN
            start = s0 - PRE - HALO
            xt = np.zeros((NTOK, D), np.float32)
            lo = max(start, 0)
            xt[lo - start:, :] = x[b, lo:s0 + MAIN, :]
            xTc = np.ascontiguousarray(xt.T.reshape(KC, P, NTOK).transpose(1, 0, 2))
            pos = (np.arange(NTOK) + start).astype(np.float32)
            ang = (pos[None, :] * freqs[:, None]).astype(np.float32)
            cs = np.cos(ang).astype(np.float32); sn = np.sin(ang).astype(np.float32)
            cosT = np.concatenate([cs, cs], 0)
            sinT = np.concatenate([sn, -sn], 0)
            halom = np.full((P, 1), 0.0 if q == 0 else 1.0, np.float32)
            m = dict(shared)
            m.update(xT=xTc, cosT=np.ascontiguousarray(cosT), sinT=np.ascontiguousarray(sinT), halom=halom)
            maps.append(m)
    return maps


_NC_CACHE = {}


DBG = False
LAST = {}


def run(inputs, PRE, MAIN, CAP, n_seq_cores, batch):
    key = (PRE, MAIN, CAP, DBG)
    if key not in _NC_CACHE:
        _NC_CACHE[key] = build_program(PRE, MAIN, CAP, dbg=DBG)
    nc = _NC_CACHE[key]
    maps = host_inputs(inputs, PRE, MAIN, CAP, n_seq_cores, batch)
    n = len(maps)
    res = run_bass_kernel_spmd(nc, maps, core_ids=list(range(n)))
    S = n_seq_cores * MAIN
    if DBG:
        LAST["res"] = res.results
    out = np.zeros((batch, S, D), np.float32)
    i = 0
    for b in range(batch):
        for q in range(n_seq_cores):
            out[b, q * MAIN:(q + 1) * MAIN, :] = np.asarray(res.results[i]["out"])
            i += 1
    return out


def kernel(**inputs):
    return run(inputs, PRE=12288, MAIN=4096, CAP=1536, n_seq_cores=4, batch=2)
```
